# Optimizing a Trainium2 kernel written in Bass

```python
import math
import jax, jax.numpy as jnp
from jax import lax
import numpy as np

D_MODEL = 1024
BATCH = 4
SEQ = 4096
DEPTH = 2

CHUNK = 64
Q_BLOCK = 128
EPS = 1e-6

N_A = DEPTH // 2
N_B = DEPTH - N_A
N_DENSE = (DEPTH + 1) // 2
N_MOE = DEPTH // 2

RET_QK_DIM = 256
RET_HEADS = D_MODEL // RET_QK_DIM
RET_V_DIM = 2 * RET_QK_DIM
RET_THETA = 10000.0

DIFF_HEAD_DIM = 64
DIFF_HEADS = D_MODEL // (2 * DIFF_HEAD_DIM)
DIFF_V_DIM = 2 * DIFF_HEAD_DIM
ROPE_THETA = 500000.0
ROPE_DIM = DIFF_HEAD_DIM // 4

FFN_DIM = 256 * math.ceil(8 * D_MODEL / 3 / 256)
N_EXPERTS = 8
TOP_K = 2
EXPERT_DIM = 7 * D_MODEL // 2

kernel_name = "yoco_retention_diffattn_moe"


def rms_norm(x, g):
    xf = x.astype(jnp.float32)
    y = xf * lax.rsqrt(jnp.mean(xf * xf, axis=-1, keepdims=True) + EPS)
    return (y * g.astype(jnp.float32)).astype(x.dtype)


def rope_tables(seq, rot_dim, theta):
    inv = 1.0 / (theta ** (jnp.arange(0, rot_dim, 2, dtype=jnp.float32) / rot_dim))
    ang = jnp.arange(seq, dtype=jnp.float32)[:, None] * inv[None, :]
    return jnp.cos(ang), jnp.sin(ang)


def apply_rope(x, cos, sin, rot_dim):
    half = rot_dim // 2
    shape = (1, x.shape[1]) + (1,) * (x.ndim - 3) + (half,)
    c = cos.reshape(shape).astype(x.dtype)
    s = sin.reshape(shape).astype(x.dtype)
    x1 = x[..., :half]
    x2 = x[..., half:rot_dim]
    return jnp.concatenate([x1 * c - x2 * s, x2 * c + x1 * s, x[..., rot_dim:]], axis=-1)


def to_chunks(t):
    b, s, h, d = t.shape
    return t.reshape(b, s // CHUNK, CHUNK, h, d).transpose(1, 0, 3, 2, 4)


def retention_mixer(h, w_in, w_o):
    b, s, _ = h.shape
    dt = h.dtype
    proj = (h @ w_in).astype(jnp.float32)
    d_qk = RET_HEADS * RET_QK_DIM
    d_v = RET_HEADS * RET_V_DIM
    q = proj[..., :d_qk].reshape(b, s, RET_HEADS, RET_QK_DIM)
    k = proj[..., d_qk:2 * d_qk].reshape(b, s, RET_HEADS, RET_QK_DIM)
    v = proj[..., 2 * d_qk:2 * d_qk + d_v].reshape(b, s, RET_HEADS, RET_V_DIM)
    g = proj[..., 2 * d_qk + d_v:]
    cos, sin = rope_tables(s, RET_QK_DIM, RET_THETA)
    q = apply_rope(q, cos, sin, RET_QK_DIM)
    k = apply_rope(k, cos, sin, RET_QK_DIM) * (RET_QK_DIM ** -0.5)

    log_gamma = jnp.log(1.0 - 2.0 ** (-5.0 - jnp.arange(RET_HEADS, dtype=jnp.float32)))
    idx = jnp.arange(CHUNK, dtype=jnp.float32)
    rel = idx[:, None] - idx[None, :]
    inner_decay = jnp.where(rel[None] >= 0,
                            jnp.exp(jnp.maximum(rel, 0.0)[None] * log_gamma[:, None, None]), 0.0)
    q_decay = jnp.exp((idx + 1.0)[None, :] * log_gamma[:, None])[None, :, :, None]
    k_decay = jnp.exp((CHUNK - 1.0 - idx)[None, :] * log_gamma[:, None])[None, :, :, None]
    chunk_decay = jnp.exp(CHUNK * log_gamma)[None, :, None, None]

    def step(state, xs):
        qc, kc, vc = xs
        scores = jnp.einsum('bhnd,bhmd->bhnm', qc, kc) * inner_decay[None]
        out = (jnp.einsum('bhnm,bhmv->bhnv', scores, vc)
               + jnp.einsum('bhnd,bhdv->bhnv', qc, state) * q_decay)
        state = state * chunk_decay + jnp.einsum('bhmd,bhmv->bhdv', kc * k_decay, vc)
        return state, out

    state0 = jnp.zeros((b, RET_HEADS, RET_QK_DIM, RET_V_DIM), jnp.float32)
    _, o = lax.scan(step, state0, (to_chunks(q), to_chunks(k), to_chunks(v)))
    o = o.transpose(1, 0, 3, 2, 4).reshape(b, s, RET_HEADS, RET_V_DIM)
    mu = jnp.mean(o, axis=-1, keepdims=True)
    var = jnp.mean(jnp.square(o - mu), axis=-1, keepdims=True)
    o = ((o - mu) * lax.rsqrt(var + EPS)).reshape(b, s, d_v)
    o = (o * jax.nn.silu(g)).astype(dt)
    return o @ w_o


def shared_kv(x, kv_norm, w_kv, cos, sin):
    b, s, _ = x.shape
    kv = rms_norm(x, kv_norm) @ w_kv
    d_k = DIFF_HEADS * 2 * DIFF_HEAD_DIM
    k = kv[..., :d_k].reshape(b, s, DIFF_HEADS, 2, DIFF_HEAD_DIM)
    k = apply_rope(k, cos, sin, ROPE_DIM).transpose(0, 2, 3, 1, 4)
    v = kv[..., d_k:].reshape(b, s, DIFF_HEADS, DIFF_V_DIM).transpose(0, 2, 1, 3)
    return k, v


def diff_attention(h, k_sh, v_sh, w_q, lq1, lk1, lq2, lk2, subln, w_o, lambda_init, cos, sin):
    b, s, _ = h.shape
    q = (h @ w_q).reshape(b, s, DIFF_HEADS, 2, DIFF_HEAD_DIM)
    q = apply_rope(q, cos, sin, ROPE_DIM)
    n_qb = s // Q_BLOCK
    qb = q.reshape(b, n_qb, Q_BLOCK, DIFF_HEADS, 2, DIFF_HEAD_DIM).transpose(1, 0, 3, 4, 2, 5)
    lam = (jnp.exp(jnp.sum(lq1.astype(jnp.float32) * lk1.astype(jnp.float32)))
           - jnp.exp(jnp.sum(lq2.astype(jnp.float32) * lk2.astype(jnp.float32))) + lambda_init)
    key_chunk = jnp.arange(s) // CHUNK
    scale = DIFF_HEAD_DIM ** -0.5
    neg = jnp.finfo(jnp.float32).min

    def block(args):
        q_blk, bi = args
        q_chunk = (bi * Q_BLOCK + jnp.arange(Q_BLOCK)) // CHUNK
        mask = key_chunk[None, :] <= q_chunk[:, None]
        sc = jnp.einsum('bhcqd,bhckd->bhcqk', q_blk, k_sh).astype(jnp.float32) * scale
        p = jax.nn.softmax(jnp.where(mask, sc, neg), axis=-1)
        a = p[:, :, 0] - lam * p[:, :, 1]
        return jnp.einsum('bhqk,bhkv->bhqv', a.astype(v_sh.dtype), v_sh)

    o = lax.map(block, (qb, jnp.arange(n_qb)))
    o = o.transpose(1, 0, 3, 2, 4).reshape(b, s, DIFF_HEADS, DIFF_V_DIM)
    o = rms_norm(o, subln) * (1.0 - lambda_init)
    return o.reshape(b, s, DIFF_HEADS * DIFF_V_DIM) @ w_o


def swiglu(h, w_gu, w_down):
    f = w_down.shape[0]
    gu = h @ w_gu
    return (jax.nn.silu(gu[..., :f]) * gu[..., f:]) @ w_down


def moe_swiglu(h, router, w_gu, w_down):
    b, s, d = h.shape
    t = h.reshape(b * s, d)
    logits = (t @ router).astype(jnp.float32)
    top_val, top_idx = lax.top_k(logits, TOP_K)
    top_w = jax.nn.softmax(top_val, axis=-1)
    gates = jnp.sum(jax.nn.one_hot(top_idx, N_EXPERTS, dtype=jnp.float32) * top_w[..., None], axis=1)
    gates = gates.astype(t.dtype)
    y = jnp.zeros_like(t)
    for e in range(N_EXPERTS):
        y = y + gates[:, e:e + 1] * swiglu(t, w_gu[e], w_down[e])
    return y.reshape(b, s, d)


def setup_inputs(seed: int = 0) -> dict:
    key = jax.random.key(seed)
    ks = jax.random.split(key, 24)
    D = D_MODEL
    def w(k, shape, fan_in):
        return jax.random.normal(k, shape, jnp.float32) * (fan_in ** -0.5)
    def gain(k, shape):
        return 1.0 + 0.01 * jax.random.normal(k, shape, jnp.float32)
    ret_in_cols = 2 * RET_HEADS * RET_QK_DIM + 2 * RET_HEADS * RET_V_DIM
    ret_v = RET_HEADS * RET_V_DIM
    diff_w = DIFF_HEADS * DIFF_V_DIM
    return {
        "x": jax.random.normal(ks[0], (BATCH, SEQ, D), jnp.float32),
        "ln_mix": gain(ks[1], (DEPTH, D)),
        "ln_ffn": gain(ks[2], (DEPTH, D)),
        "ret_w_in": w(ks[3], (N_A, D, ret_in_cols), D),
        "ret_w_o": w(ks[4], (N_A, ret_v, D), ret_v),
        "kv_norm": gain(ks[5], (D,)),
        "w_kv": w(ks[6], (D, DIFF_HEADS * 2 * DIFF_HEAD_DIM + diff_w), D),
        "diff_w_q": w(ks[7], (N_B, D, DIFF_HEADS * 2 * DIFF_HEAD_DIM), D),
        "lam_q1": 0.1 * jax.random.normal(ks[8], (N_B, DIFF_HEAD_DIM), jnp.float32),
        "lam_k1": 0.1 * jax.random.normal(ks[9], (N_B, DIFF_HEAD_DIM), jnp.float32),
        "lam_q2": 0.1 * jax.random.normal(ks[10], (N_B, DIFF_HEAD_DIM), jnp.float32),
        "lam_k2": 0.1 * jax.random.normal(ks[11], (N_B, DIFF_HEAD_DIM), jnp.float32),
        "diff_subln": gain(ks[12], (N_B, DIFF_V_DIM)),
        "diff_w_o": w(ks[13], (N_B, diff_w, D), diff_w),
        "ffn_w_gu": w(ks[14], (N_DENSE, D, 2 * FFN_DIM), D),
        "ffn_w_down": w(ks[15], (N_DENSE, FFN_DIM, D), FFN_DIM),
        "moe_router": w(ks[16], (N_MOE, D, N_EXPERTS), D),
        "moe_w_gu": w(ks[17], (N_MOE, N_EXPERTS, D, 2 * EXPERT_DIM), D),
        "moe_w_down": w(ks[18], (N_MOE, N_EXPERTS, EXPERT_DIM, D), EXPERT_DIM),
        "final_norm": gain(ks[19], (D,)),
    }


def reference(x, ln_mix, ln_ffn, ret_w_in, ret_w_o, kv_norm, w_kv, diff_w_q, lam_q1, lam_k1,
              lam_q2, lam_k2, diff_subln, diff_w_o, ffn_w_gu, ffn_w_down, moe_router, moe_w_gu,
              moe_w_down, final_norm):
    s = x.shape[1]
    cos, sin = rope_tables(s, ROPE_DIM, ROPE_THETA)
    k_sh, v_sh = (shared_kv(x, kv_norm, w_kv, cos, sin) if N_A == 0 else (None, None))
    for layer in range(DEPTH):
        h = rms_norm(x, ln_mix[layer])
        if layer < N_A:
            x = x + retention_mixer(h, ret_w_in[layer], ret_w_o[layer])
        else:
            bl = layer - N_A
            lambda_init = 0.8 - 0.6 * math.exp(-0.3 * layer)
            x = x + diff_attention(h, k_sh, v_sh, diff_w_q[bl], lam_q1[bl], lam_k1[bl],
                                   lam_q2[bl], lam_k2[bl], diff_subln[bl], diff_w_o[bl],
                                   lambda_init, cos, sin)
        h = rms_norm(x, ln_ffn[layer])
        if layer % 2 == 0:
            x = x + swiglu(h, ffn_w_gu[layer // 2], ffn_w_down[layer // 2])
        else:
            x = x + moe_swiglu(h, moe_router[layer // 2], moe_w_gu[layer // 2], moe_w_down[layer // 2])
        if layer == N_A - 1:
            k_sh, v_sh = shared_kv(x, kv_norm, w_kv, cos, sin)
    return rms_norm(x, final_norm)
```

```python
import bisect
import math
from contextlib import ExitStack

import numpy as np
import concourse.bass as bass
import concourse.mybir as mybir
from concourse.bass_utils import run_bass_kernel_spmd

F32 = mybir.dt.float32
BF16 = mybir.dt.bfloat16
AF = mybir.ActivationFunctionType
ALU = mybir.AluOpType

ENGS = ("pe", "act", "dve", "pool", "sp")
D = 1024
S = 4096
NCORE = 8
EPS = 1e-6
FFN = 2816
EXD = 3584
NEXP = 8
LAMBDA_INIT = 0.8 - 0.6 * math.exp(-0.3 * 1)


class Reg:
    __slots__ = ("name", "w", "r", "dsem", "dcnt")

    def __init__(self, name=""):
        self.name = name
        self.w = None
        self.r = []
        self.dsem = None
        self.dcnt = 0


class KB:
    def __init__(self, nc, es):
        self.nc = nc
        self.es = es
        self.eng = {"pe": nc.tensor, "act": nc.scalar, "dve": nc.vector,
                    "pool": nc.gpsimd, "sp": nc.sync}
        self.sem = {e: es.enter_context(nc.semaphore("c_" + e)) for e in ENGS}
        self.nsig = {e: 0 for e in ENGS}
        self.seq = {e: 0 for e in ENGS}
        self.last = {e: None for e in ENGS}
        self.sig_seq = {e: [] for e in ENGS}
        self.sig_snap = {e: [] for e in ENGS}
        self.seen = {e: {e2: 0 for e2 in ENGS} for e in ENGS}
        self.seen_d = {e: {} for e in ENGS}
        self.snap_last = {e: None for e in ENGS}
        self.dma_regs = []
        self.nwaits = 0
        self.nsem = 0
        self.nops = {e: 0 for e in ENGS}

    def sb(self, name, shape, dt, es=None):
        return (es or self.es).enter_context(self.nc.sbuf_tensor("s_" + name, list(shape), dt))

    def ps(self, name, shape, dt, es=None):
        return (es or self.es).enter_context(self.nc.psum_tensor("p_" + name, list(shape), dt))

    def dram(self, name, shape, dt, kind="Internal"):
        return self.nc.dram_tensor(name, list(shape), dt, kind=kind).ap()

    def _token_for(self, e, seq):
        ss = self.sig_seq[e]
        i = bisect.bisect_left(ss, seq)
        if i < len(ss):
            return i + 1
        ins = self.last[e]
        assert ins is not None and self.seq[e] >= seq, (e, seq, self.seq[e])
        ins.then_inc(self.sem[e], 1)
        self.nsig[e] += 1
        ss.append(self.seq[e])
        self.sig_snap[e].append(self.snap_last[e])
        self.last[e] = None
        return self.nsig[e]

    def _wait(self, e, tok):
        if tok is None:
            return
        if tok[0] == "c":
            _, e2, seq = tok
            if e2 == e and e == "pe":
                return
            ss = self.sig_seq[e2]
            known = self.seen[e][e2]
            if known > 0 and ss[known - 1] >= seq:
                return
            n = self._token_for(e2, seq)
            if self.seen[e][e2] >= n:
                return
            self.eng[e].wait_ge(self.sem[e2], n)
            self.nwaits += 1
            self.seen[e][e2] = n
            snap = self.sig_snap[e2][n - 1]
            if snap is not None:
                for e3, v in snap.items():
                    if v > self.seen[e][e3]:
                        self.seen[e][e3] = v
        else:
            _, sem, cnt, sid = tok
            if self.seen_d[e].get(sid, 0) >= cnt:
                return
            self.eng[e].wait_ge(sem, cnt)
            self.nwaits += 1
            self.seen_d[e][sid] = cnt

    def _deps(self, e, rd, wr):
        for r in rd:
            self._wait(e, r.w)
        for r in wr:
            self._wait(e, r.w)
            for t in r.r:
                self._wait(e, t)

    def op(self, e, fn, rd=(), wr=()):
        self._deps(e, rd, wr)
        ins = fn(self.eng[e])
        self.seq[e] += 1
        self.nops[e] += 1
        self.last[e] = ins
        self.snap_last[e] = dict(self.seen[e])
        tok = ("c", e, self.seq[e])
        for r in rd:
            r.r.append(tok)
        for r in wr:
            r.w = tok
            r.r = []
        return ins

    def dma(self, q, out, in_, rd=(), wr=(), **kw):
        self._deps(q, rd, wr)
        d = wr[0]
        if d.dsem is None:
            d.dsem = self.es.enter_context(self.nc.semaphore("d%d" % self.nsem))
            self.nsem += 1
            self.dma_regs.append(d)
        ins = self.eng[q].dma_start(out=out, in_=in_, **kw)
        ins.then_inc(d.dsem, 16)
        d.dcnt += 16
        tok = ("d", d.dsem, d.dcnt, id(d))
        for r in rd:
            r.r.append(tok)
        for r in wr:
            r.w = tok
            r.r = []
        return ins

    def barrier(self):
        sp = "sp"
        for d in self.dma_regs:
            if d.dcnt:
                self._wait(sp, ("d", d.dsem, d.dcnt, id(d)))
        for e in ENGS:
            if e != sp and self.seq[e] > 0:
                self._wait(sp, ("c", e, self.seq[e]))
        self.eng[sp].sem_inc(self.sem[sp], 1)
        self.nsig[sp] += 1
        self.seq[sp] += 1
        self.sig_seq[sp].append(self.seq[sp])
        self.sig_snap[sp].append(dict(self.seen[sp]))
        self.last[sp] = None
        n = self.nsig[sp]
        for e in ENGS:
            if e == sp:
                continue
            self.eng[e].wait_ge(self.sem[sp], n)
            self.seen[e][sp] = n
            for e3, v in self.seen[sp].items():
                if v > self.seen[e][e3]:
                    self.seen[e][e3] = v
            for sid, v in self.seen_d[sp].items():
                if v > self.seen_d[e].get(sid, 0):
                    self.seen_d[e][sid] = v

    def finish(self, out_regs):
        for d in out_regs:
            self._wait("sp", ("d", d.dsem, d.dcnt, id(d)))


class WStream:
    def __init__(self, k, specs, es, nbuf=3, pf=2, q="pool", name="wb"):
        self.k = k
        self.specs = specs
        self.nbuf = nbuf
        self.pf = pf
        self.q = q
        self.bufs = [k.sb("%s%d" % (name, i), [128, 8, 512], BF16, es) for i in range(nbuf)]
        self.regs = [Reg() for _ in range(nbuf)]
        self.issued = 0
        self.cur = 0

    def _issue(self, i):
        buf = self.bufs[i % self.nbuf]
        reg = self.regs[i % self.nbuf]
        for (kc0, c0, src, nk, ncols) in self.specs[i][1]:
            self.k.dma(self.q, buf[:, kc0:kc0 + nk, c0:c0 + ncols],
                       src.rearrange("(kc p) n -> p kc n", p=128), wr=[reg])

    def next(self, tag):
        i = self.cur
        assert self.specs[i][0] == tag, (self.specs[i][0], tag)
        lim = min(len(self.specs), i + 1 + self.pf)
        while self.issued < lim:
            self._issue(self.issued)
            self.issued += 1
        self.cur += 1
        return self.bufs[i % self.nbuf], self.regs[i % self.nbuf]


def host_consts():
    c = {}
    inv = (1.0 / (np.float32(10000.0) ** (np.arange(0, 256, 2, dtype=np.float32) / np.float32(256)))).astype(np.float32)
    pos = np.arange(S, dtype=np.float32)
    ang = (pos[:, None] * inv[None, :]).astype(np.float32)
    c["rcos"] = np.ascontiguousarray(np.cos(ang).astype(np.float32).T)
    c["rsin"] = np.ascontiguousarray(np.sin(ang).astype(np.float32).T)
    inv2 = (1.0 / (np.float32(500000.0) ** (np.arange(0, 16, 2, dtype=np.float32) / np.float32(16)))).astype(np.float32)
    ang2 = (pos[:, None] * inv2[None, :]).astype(np.float32)
    c["dcs"] = np.concatenate([np.tile(np.cos(ang2), (1, 8)), np.tile(np.sin(ang2), (1, 8))], axis=1).astype(np.float32)
    cst = np.zeros((128, 32), np.float32)
    maskT = np.zeros((128, 4, 128), np.float32)
    n = np.arange(128, dtype=np.float64)
    cds = []
    for h in range(4):
        g = 1.0 - 2.0 ** (-5.0 - h)
        qd = g ** (n + 1.0)
        cst[:, h] = qd
        cst[:, 4 + h] = g ** (127.0 - n) / 16.0
        cst[:, 8 + h] = qd * qd
        m = n[:, None]
        nn = n[None, :]
        maskT[:, h, :] = np.where(nn >= m, g ** (-(m + 1.0)) / 16.0, 0.0)
        cds.append(float(g ** 128.0))
    cst[:, 12] = -0.5
    cst[:, 13] = EPS
    c["cst"] = cst
    c["maskT"] = maskT
    c["cds"] = cds
    c["ident"] = np.eye(128, dtype=np.float32)
    return c


class _Stop(Exception):
    pass


def build_program(ntiles=8, phases="ABC", debug=False, limit=10**9):
    def stage(n):
        if n > limit:
            raise _Stop()

    nc = bass.Bass("TRN2", target_bir_lowering=False)
    HC = host_consts()
    cds = HC["cds"]

    declared = []

    def inp(name, shape, ph, dt=F32):
        if not (set(ph) & set(phases)):
            return None
        declared.append(name)
        return nc.dram_tensor(name, list(shape), dt, kind="ExternalInput").ap()

    xin = inp("x", [S, D], "A")
    ln_mix = inp("ln_mix", [2, D], "AB")
    ln_ffn = inp("ln_ffn", [2, D], "AC")
    ret_w_in = inp("ret_w_in", [D, 6144], "A")
    ret_w_o = inp("ret_w_o", [2048, D], "A")
    kv_norm = inp("kv_norm", [1, D], "A")
    w_kv = inp("w_kv", [D, 2048], "A")
    w_q = inp("diff_w_q", [D, D], "B")
    lam = inp("lam", [1, 256], "B")
    subln = inp("diff_subln", [1, 128], "B")
    w_do = inp("diff_w_o", [D, D], "B")
    ffn_gu = inp("ffn_w_gu", [D, 2 * FFN], "A")
    ffn_dn = inp("ffn_w_down", [FFN, D], "A")
    router = inp("moe_router", [D, NEXP], "C")
    moe_gu = inp("moe_w_gu", [NEXP, D, 2 * EXD], "C")
    moe_dn = inp("moe_w_down", [NEXP, EXD, D], "C")
    final_norm = inp("final_norm", [1, D], "C")
    rcos = inp("rcos", [128, S], "A")
    rsin = inp("rsin", [128, S], "A")
    dcs = inp("dcs", [S, 128], "A")
    dcs_own = inp("dcs_own", [2048, 128], "B")
    cst_in = inp("cst", [128, 32], "ABC")
    maskT_in = inp("maskT", [128, 4, 128], "A")
    ident_in = inp("ident", [128, 128], "ABC")
    amask_in = inp("amask", [128, 2, 128], "B")
    nc_in_names = declared

    out = nc.dram_tensor("out", [2048, D], F32, kind="ExternalOutput").ap() if "C" in phases else None
    dbg_outs = []

    def scratch(name, shape, dt, prod, cons):
        if prod in phases:
            kind = "ExternalOutput" if debug else "Internal"
        elif cons in phases:
            kind = "ExternalInput"
            declared.append(name)
        else:
            return None
        return nc.dram_tensor(name, list(shape), dt, kind=kind).ap()

    x1s = scratch("x1s", [S, D], F32, "A", "B")
    kTs = scratch("kTs", [8, 128, S], BF16, "A", "B")
    vs = scratch("vs", [S, D], BF16, "A", "B")
    xbs = scratch("xbs", [2048, D], F32, "B", "C")
    xown = nc.dram_tensor("xown", [2048, D], F32, kind="Internal").ap() if "B" in phases else None

    with ExitStack() as es:
        k = KB(nc, es)
        R_x1s = [Reg() for _ in range(8)]
        R_kTs = [Reg() for _ in range(8)]
        R_vs = [Reg() for _ in range(8)]
        R_xbs = [Reg() for _ in range(4)]
        R_xown = [Reg() for _ in range(4)]
        R_out = Reg()

        cst = k.sb("cst", [128, 32], F32); R_cst = Reg()
        ident = k.sb("ident", [128, 128], BF16); R_ident = Reg()
        k.dma("sp", cst[:], cst_in[:, :], wr=[R_cst])
        k.dma("pool", ident[:], ident_in[:, :], wr=[R_ident])

        PB = [k.ps("pb%d" % i, [128, 512], F32) for i in range(7)]
        R_PB = [Reg() for _ in range(7)]
        PT = k.ps("pt", [128, 1024], BF16); R_PT = Reg()

        def bcast_load(dst, src_row, reg, q="sp"):
            k.dma(q, dst, src_row.partition_broadcast(128), wr=[reg])

        def rmsnorm_to_hT(src_ap, R_src, gain, R_gain, hT_dst, R_hT, tmp, defer=False, sq_on_dve=False):
            (junk, R_junk, ss, R_ss, ve, R_ve, rstd, R_rstd, hn, R_hn) = tmp
            if sq_on_dve:
                k.op("dve", lambda e: e.scalar_tensor_tensor(out=junk[:], in0=src_ap, scalar=1.0, in1=src_ap,
                                                             op0=ALU.mult, op1=ALU.mult, accum_out=ss[:, 0:1]),
                     rd=[R_src], wr=[R_junk, R_ss])
            else:
                k.op("act", lambda e: e.activation(out=junk[:], in_=src_ap, func=AF.Square,
                                                   accum_out=ss[:, 0:1]), rd=[R_src], wr=[R_junk, R_ss])
            k.op("dve", lambda e: e.tensor_scalar(out=ve[:], in0=ss[:], scalar1=1.0 / D, scalar2=EPS,
                                                  op0=ALU.mult, op1=ALU.add), rd=[R_ss], wr=[R_ve])
            k.op("pool", lambda e: e.tensor_tensor(out=rstd[:], in0=ve[:], in1=cst[:, 12:13], op=ALU.pow),
                 rd=[R_ve, R_cst], wr=[R_rstd])
            k.op("dve", lambda e: e.scalar_tensor_tensor(out=hn[:], in0=src_ap, scalar=rstd[:, 0:1], in1=gain,
                                                         op0=ALU.mult, op1=ALU.mult),
                 rd=[R_src, R_rstd, R_gain], wr=[R_hn])
            def back():
                for c in range(8):
                    k.op("pe", lambda e, c=c: e.transpose(out=PT[:, c * 128:(c + 1) * 128],
                                                          in_=hn[:, c * 128:(c + 1) * 128], identity=ident[:]),
                         rd=[R_hn, R_ident], wr=[R_PT])
                k.op("act", lambda e: e.activation(out=hT_dst, in_=PT[:].rearrange("p (c t) -> p c t", c=8),
                                                   func=AF.Copy), rd=[R_PT], wr=[R_hT])
            if defer:
                return back
            back()

        if "A" in phases:
            with ExitStack() as pes:
              try:
                    specs = []
                    for t in range(ntiles):
                        for hd in range(4):
                            specs.append((("qk", t, hd), [(0, 0, ret_w_in[:, hd * 256:(hd + 1) * 256], 8, 256),
                                                          (0, 256, ret_w_in[:, 1024 + hd * 256:1024 + (hd + 1) * 256], 8, 256)]))
                            specs.append((("v", t, hd), [(0, 0, ret_w_in[:, 2048 + hd * 512:2048 + (hd + 1) * 512], 8, 512)]))
                            specs.append((("g", t, hd), [(0, 0, ret_w_in[:, 4096 + hd * 512:4096 + (hd + 1) * 512], 8, 512)]))
                        if t > 0:
                            for nci in range(4):
                                specs.append((("kv", t - 1, nci), [(0, 0, w_kv[:, nci * 512:(nci + 1) * 512], 8, 512)]))
                        for nh in range(2):
                            for kg in range(2):
                                specs.append((("wo", t, nh, kg),
                                              [(0, 0, ret_w_o[kg * 1024:(kg + 1) * 1024, nh * 512:(nh + 1) * 512], 8, 512)]))
                        for j in range(11):
                            specs.append((("gu", t, j), [(0, 0, ffn_gu[:, j * 256:(j + 1) * 256], 8, 256),
                                                         (0, 256, ffn_gu[:, FFN + j * 256:FFN + (j + 1) * 256], 8, 256)]))
                        for nh in range(2):
                            for kg, (k0, nk) in enumerate(((0, 8), (8, 8), (16, 6))):
                                specs.append((("dn", t, nh, kg),
                                              [(0, 0, ffn_dn[k0 * 128:(k0 + nk) * 128, nh * 512:(nh + 1) * 512], nk, 512)]))
                        if t == ntiles - 1:
                            for nci in range(4):
                                specs.append((("kv", t, nci), [(0, 0, w_kv[:, nci * 512:(nci + 1) * 512], 8, 512)]))
                    ws = WStream(k, specs, pes, nbuf=4, pf=3)

                    xtb = [k.sb("xt%d" % i, [128, 4, D], F32, pes) for i in range(2)]
                    R_xtb = [[Reg() for _ in range(4)] for _ in range(2)]
                    hTb = [k.sb("hT%d" % i, [128, 8, 512], BF16, pes) for i in range(2)]
                    R_hTb = [[Reg() for _ in range(4)] for _ in range(2)]
                    xt, R_xt, hT, R_hT = xtb[0], R_xtb[0], hTb[0], R_hTb[0]
                    qk4 = k.sb("qk4", [128, 4, 2, 512], BF16, pes)
                    qT2 = [qk4[:, i, :, :] for i in range(2)]; R_qT2 = [Reg(), Reg()]
                    kT2 = [qk4[:, 2 + i, :, :] for i in range(2)]; R_kT2 = [Reg(), Reg()]
                    ktm2 = [k.sb("ktm%d" % i, [128, 4, 256], BF16, pes) for i in range(2)]; R_ktm2 = [Reg(), Reg()]
                    vtm2 = [k.sb("vtm%d" % i, [128, 4, 512], BF16, pes) for i in range(2)]
                    R_vtm2 = [[Reg() for _ in range(4)] for _ in range(2)]
                    sg2 = [k.sb("sg%d" % i, [128, 4, 512], BF16, pes) for i in range(2)]
                    R_sg2 = [[Reg() for _ in range(4)] for _ in range(2)]
                    st = k.sb("st", [128, 8, 512], F32, pes); R_st = [Reg() for _ in range(8)]
                    stb = k.sb("stb", [128, 8, 512], BF16, pes); R_stb = [Reg() for _ in range(8)]
                    actT = k.sb("actT", [128, 22, 512], BF16, pes); R_actT = Reg()
                    ogT = actT[:, 0:16, :]; R_ogT = R_actT
                    gains = k.sb("gains", [128, 3, D], F32, pes); R_gains = Reg()
                    maskT = k.sb("maskT", [128, 4, 128], F32, pes); R_maskT = Reg()
                    cs = k.sb("cs", [128, 2, 512], F32, pes); R_cs = Reg()
                    dcsT = k.sb("dcsT", [128, 4, 128], F32, pes); R_dcsT = Reg()
                    rt = [k.sb("rt%d" % i, [128, 512], F32, pes) for i in range(4)]; R_rt = [Reg() for _ in range(4)]
                    scT = k.sb("scT", [128, 128], BF16, pes); R_scT = Reg()
                    on = k.sb("on", [128, 512], F32, pes); R_on = Reg()
                    og2 = [k.sb("og%d" % i, [128, 512], BF16, pes) for i in range(2)]; R_og2 = [Reg(), Reg()]
                    sgf2 = [k.sb("sgf%d" % i, [128, 512], BF16, pes) for i in range(2)]; R_sgf2 = [Reg(), Reg()]
                    kk2 = [k.sb("kk%d" % i, [128, 512], BF16, pes) for i in range(2)]; R_kk2 = [Reg(), Reg()]
                    kr2 = [[k.sb("kr%d_%d" % (j, i), [128, 8, 8], F32, pes) for i in range(4)] for j in range(2)]
                    R_kr2 = [[Reg() for _ in range(4)] for _ in range(2)]
                    kst2 = [k.sb("kst%d" % i, [128, 4, 128], BF16, pes) for i in range(2)]; R_kst2 = [Reg(), Reg()]
                    vst2 = [k.sb("vst%d" % i, [128, 512], BF16, pes) for i in range(2)]; R_vst2 = [Reg(), Reg()]
                    sm = k.sb("sm", [128, 16], F32, pes); R_sm = [Reg() for _ in range(16)]
                    tmps = []
                    junk_sh = k.sb("junk", [128, D], BF16, pes)
                    for i in range(2):
                        junk_i = junk_sh
                        hn_i = k.sb("hn%d" % i, [128, D], BF16, pes)
                        smn = k.sb("smn%d" % i, [128, 4], F32, pes)
                        tmps.append((junk_i, Reg(), smn[:, 0:1], Reg(), smn[:, 1:2], Reg(), smn[:, 2:3], Reg(), hn_i, Reg()))

                    print('SBUF remaining phase A', nc.sbuf_bytes_remaining)
                    bcast_load(gains[:, 0, :], ln_mix[0:1, :], R_gains)
                    bcast_load(gains[:, 1, :], ln_ffn[0:1, :], R_gains)
                    bcast_load(gains[:, 2, :], kv_norm[0:1, :], R_gains)
                    k.dma("sp", maskT[:], maskT_in[:, :, :], wr=[R_maskT])
                    k.op("dve", lambda e: e.memset(st[:], 0.0), wr=R_st)
                    k.op("pool", lambda e: e.memset(stb[:], 0.0), wr=R_stb)
                    stage(1)

                    def gemm_T_acc(tagbase, lhs_of, R_lhs, kgroups, post):
                        for nh in range(2):
                            for kg, nk in enumerate(kgroups):
                                wb, R_w = ws.next(tagbase + (nh, kg))
                                k0 = sum(kgroups[:kg])
                                for b in range(4):
                                    for kc in range(nk):
                                        first = (kg == 0 and kc == 0)
                                        lastm = (kg == len(kgroups) - 1 and kc == nk - 1)
                                        k.op("pe", lambda e, b=b, kc=kc, first=first, lastm=lastm, wb=wb, k0=k0:
                                             e.matmul(PB[b][:], lhsT=lhs_of(k0 + kc, b), rhs=wb[:, kc, :],
                                                      start=first, stop=lastm),
                                             rd=[R_w] + R_lhs, wr=[R_PB[b]])
                            for b in range(4):
                                post(nh, b, PB[b], R_PB[b])

                    def resid_add(nh, b, P, R_P):
                        k.op("dve", lambda e: e.tensor_tensor(out=xt[:, b, nh * 512:(nh + 1) * 512],
                                                              in0=xt[:, b, nh * 512:(nh + 1) * 512], in1=P[:], op=ALU.add),
                             rd=[R_P, R_xt[b]], wr=[R_xt[b]])

                    def load_x(tt):
                        k.dma("sp", xtb[tt % 2][:], xin[tt * 512:(tt + 1) * 512, :].rearrange("(b p) d -> p b d", p=128),
                              wr=R_xtb[tt % 2])

                    def load_cs(tt):
                        k.dma("sp", cs[:, 0, :], rcos[:, tt * 512:(tt + 1) * 512], wr=[R_cs])
                        k.dma("sp", cs[:, 1, :], rsin[:, tt * 512:(tt + 1) * 512], wr=[R_cs])

                    def load_dcs(tt):
                        k.dma("sp", dcsT[:], dcs[tt * 512:(tt + 1) * 512, :].rearrange("(b p) d -> p b d", p=128), wr=[R_dcsT])

                    def norm0(tt):
                        for b in range(4):
                            rmsnorm_to_hT(xtb[tt % 2][:, b, :], R_xtb[tt % 2][b], gains[:, 0, :], R_gains,
                                          hTb[tt % 2][:, :, b * 128:(b + 1) * 128], R_hTb[tt % 2][b], tmps[b % 2])

                    kv_pending = []
                    load_x(0)
                    load_cs(0)
                    load_dcs(0)
                    norm0(0)
                    for t in range(ntiles):
                        ts0 = t * 512
                        xt, R_xt, hT, R_hT = xtb[t % 2], R_xtb[t % 2], hTb[t % 2], R_hTb[t % 2]
                        if t + 1 < ntiles:
                            load_x(t + 1)
                        stage(2)
                        stage(3)
                        def make_pieces(hd):
                            pi = hd % 2
                            qTc, kTc, ktmc, vtmc, sgc_ = qT2[pi], kT2[pi], ktm2[pi], vtm2[pi], sg2[pi]
                            R_qTc, R_kTc, R_ktmc, R_vtmc, R_sgc_ = R_qT2[pi], R_kT2[pi], R_ktm2[pi], R_vtm2[pi], R_sg2[pi]
                            stt = {}

                            def proj_mm(which, dc, bank):
                                wb, R_w = stt["w"]
                                P, R_P = PB[bank], R_PB[bank]
                                for kc in range(8):
                                    c0 = which * 256 + dc * 128
                                    k.op("pe", lambda e, kc=kc, c0=c0: e.matmul(P[:], lhsT=wb[:, kc, c0:c0 + 128], rhs=hT[:, kc, :],
                                                                                start=(kc == 0), stop=(kc == 7)),
                                         rd=[R_w] + R_hT, wr=[R_P])

                            def rope_ops(dst, R_dst, ba, bb):
                                Pa, Pb, R_Pa, R_Pb = PB[ba], PB[bb], R_PB[ba], R_PB[bb]
                                k.op("dve", lambda e: e.tensor_tensor(out=rt[0][:], in0=Pa[:], in1=cs[:, 0, :], op=ALU.mult),
                                     rd=[R_Pa, R_cs], wr=[R_rt[0]])
                                k.op("dve", lambda e: e.tensor_tensor(out=rt[1][:], in0=Pb[:], in1=cs[:, 1, :], op=ALU.mult),
                                     rd=[R_Pb, R_cs], wr=[R_rt[1]])
                                k.op("dve", lambda e: e.tensor_tensor(out=rt[2][:], in0=Pb[:], in1=cs[:, 0, :], op=ALU.mult),
                                     rd=[R_Pb, R_cs], wr=[R_rt[2]])
                                k.op("dve", lambda e: e.tensor_tensor(out=rt[3][:], in0=Pa[:], in1=cs[:, 1, :], op=ALU.mult),
                                     rd=[R_Pa, R_cs], wr=[R_rt[3]])
                                k.op("pool", lambda e: e.tensor_tensor(out=dst[:, 0, :], in0=rt[0][:], in1=rt[1][:], op=ALU.subtract),
                                     rd=[R_rt[0], R_rt[1]], wr=[R_dst])
                                k.op("pool", lambda e: e.tensor_tensor(out=dst[:, 1, :], in0=rt[2][:], in1=rt[3][:], op=ALU.add),
                                     rd=[R_rt[2], R_rt[3]], wr=[R_dst])

                            def h_q0():
                                stt["w"] = ws.next(("qk", t, hd))
                                proj_mm(0, 0, 0)

                            def h_q1():
                                proj_mm(0, 1, 1)
                                rope_ops(qTc, R_qTc, 0, 1)

                            def h_k0():
                                proj_mm(1, 0, 2)

                            def h_k1():
                                proj_mm(1, 1, 0)
                                rope_ops(kTc, R_kTc, 2, 0)

                            def ktm_transposes():
                                for b in range(4):
                                    for dc in range(2):
                                        k.op("pe", lambda e, b=b, dc=dc: e.transpose(
                                            out=PT[:, b * 256 + dc * 128: b * 256 + (dc + 1) * 128],
                                            in_=kTc[:, dc, b * 128:(b + 1) * 128], identity=ident[:]),
                                            rd=[R_kTc, R_ident], wr=[R_PT])
                                k.op("act", lambda e: e.activation(out=ktmc[:].rearrange("p b d -> p (b d)"), in_=PT[:],
                                                                   func=AF.Copy, scale=cst[:, 4 + hd:5 + hd]),
                                     rd=[R_PT, R_cst], wr=[R_ktmc])

                            def half_vg(tag, dstt, R_d, fn, half):
                                def f():
                                    if half == 0:
                                        stt[tag] = ws.next((tag, t, hd))
                                    if tag == "g" and half == 0:
                                        ktm_transposes()
                                    wb, R_w = stt[tag]
                                    for b in (0, 1) if half == 0 else (2, 3):
                                        bi = (b + (2 if tag == "g" else 0)) % 3
                                        P, R_P = PB[bi], R_PB[bi]
                                        for kc in range(8):
                                            k.op("pe", lambda e, b=b, kc=kc, P=P: e.matmul(P[:], lhsT=hT[:, kc, b * 128:(b + 1) * 128],
                                                                                           rhs=wb[:, kc, :], start=(kc == 0), stop=(kc == 7)),
                                                 rd=[R_w, R_hT[b]], wr=[R_P])
                                        k.op("act", lambda e, b=b, P=P: e.activation(out=dstt[:, b, :], in_=P[:], func=fn),
                                             rd=[R_P], wr=[R_d[b]])
                                return f
                            return [h_q0, h_q1, h_k0, h_k1,
                                    half_vg("v", vtmc, R_vtmc, AF.Copy, 0), half_vg("v", vtmc, R_vtmc, AF.Copy, 1),
                                    half_vg("g", sgc_, R_sgc_, AF.Silu, 0), half_vg("g", sgc_, R_sgc_, AF.Silu, 1)]

                        def rec_sd(hd, b):
                            pi = hd % 2
                            qTc, kTc, ktmc, vtmc = qT2[pi], kT2[pi], ktm2[pi], vtm2[pi]
                            R_qTc, R_kTc, R_ktmc, R_vtmc = R_qT2[pi], R_kT2[pi], R_ktm2[pi], R_vtm2[pi]
                            bs = slice(b * 128, (b + 1) * 128)
                            Psc, R_Psc = PB[3], R_PB[3]
                            Po, R_Po = PB[4], R_PB[4]
                            for dc in range(2):
                                k.op("pe", lambda e, dc=dc: e.matmul(Psc[:, 0:128], lhsT=kTc[:, dc, bs], rhs=qTc[:, dc, bs],
                                                                     start=(dc == 0), stop=(dc == 1)),
                                     rd=[R_kTc, R_qTc], wr=[R_Psc])
                            k.op("dve", lambda e: e.tensor_tensor(out=scT[:], in0=Psc[:, 0:128], in1=maskT[:, hd, :], op=ALU.mult),
                                 rd=[R_Psc, R_maskT], wr=[R_scT])
                            for dc in range(2):
                                Pd, R_Pd = PB[5 + dc], R_PB[5 + dc]
                                k.op("pe", lambda e, dc=dc, Pd=Pd: e.matmul(Pd[:], lhsT=ktmc[:, b, dc * 128:(dc + 1) * 128],
                                                                            rhs=vtmc[:, b, :], start=True, stop=True),
                                     rd=[R_ktmc, R_vtmc[b]], wr=[R_Pd])

                        def rec_o(hd, b):
                            pi = hd % 2
                            qTc, kTc, ktmc, vtmc = qT2[pi], kT2[pi], ktm2[pi], vtm2[pi]
                            R_qTc, R_kTc, R_ktmc, R_vtmc = R_qT2[pi], R_kT2[pi], R_ktm2[pi], R_vtm2[pi]
                            bs = slice(b * 128, (b + 1) * 128)
                            Po, R_Po = PB[4], R_PB[4]
                            k.op("pe", lambda e: e.matmul(Po[:], lhsT=scT[:], rhs=vtmc[:, b, :], start=True, stop=False),
                                 rd=[R_scT, R_vtmc[b]], wr=[R_Po])
                            for dc in range(2):
                                k.op("pe", lambda e, dc=dc: e.matmul(Po[:], lhsT=qTc[:, dc, bs], rhs=stb[:, hd * 2 + dc, :],
                                                                     start=False, stop=(dc == 1)),
                                     rd=[R_qTc, R_stb[hd * 2 + dc]], wr=[R_Po])
                            k.op("dve", lambda e: e.bn_stats(out=sm[:, 3:9], in_=Po[:]), rd=[R_Po], wr=[R_sm[3]])
                            k.op("dve", lambda e: e.bn_aggr(out=sm[:, 9:11], in_=sm[:, 3:9]), rd=[R_sm[3]], wr=[R_sm[4]])
                            k.op("dve", lambda e: e.tensor_scalar(out=sm[:, 11:12], in0=sm[:, 10:11],
                                                                  scalar1=cst[:, 8 + hd:9 + hd], scalar2=EPS,
                                                                  op0=ALU.mult, op1=ALU.add),
                                 rd=[R_sm[4], R_cst], wr=[R_sm[5]])
                            k.op("pool", lambda e: e.tensor_tensor(out=sm[:, 12:13], in0=sm[:, 11:12], in1=cst[:, 12:13],
                                                                   op=ALU.pow), rd=[R_sm[5], R_cst], wr=[R_sm[6]])
                            k.op("dve", lambda e: e.tensor_tensor(out=sm[:, 13:14], in0=sm[:, 12:13],
                                                                  in1=cst[:, hd:hd + 1], op=ALU.mult),
                                 rd=[R_sm[6], R_cst], wr=[R_sm[7]])
                            k.op("dve", lambda e: e.scalar_tensor_tensor(out=sm[:, 14:15], in0=sm[:, 9:10], scalar=-1.0,
                                                                         in1=sm[:, 13:14], op0=ALU.mult, op1=ALU.mult),
                                 rd=[R_sm[4], R_sm[7]], wr=[R_sm[8]])
                            k.op("act", lambda e: e.activation(out=on[:], in_=Po[:], func=AF.Identity,
                                                               bias=sm[:, 14:15], scale=sm[:, 13:14]),
                                 rd=[R_Po, R_sm[7], R_sm[8]], wr=[R_on])
                            k.op("pool", lambda e: e.tensor_tensor(out=og2[b % 2][:], in0=on[:], in1=sg2[pi][:, b, :], op=ALU.mult),
                                 rd=[R_on, R_sg2[pi][b]], wr=[R_og2[b % 2]])
                            for dc in range(2):
                                Pd, R_Pd = PB[5 + dc], R_PB[5 + dc]
                                si = hd * 2 + dc
                                k.op("dve", lambda e, si=si, Pd=Pd: e.scalar_tensor_tensor(
                                    out=st[:, si, :], in0=st[:, si, :], scalar=cds[hd], in1=Pd[:],
                                    op0=ALU.mult, op1=ALU.add), rd=[R_Pd, R_st[si]], wr=[R_st[si]])
                                k.op("act", lambda e, si=si: e.activation(out=stb[:, si, :], in_=st[:, si, :], func=AF.Copy),
                                     rd=[R_st[si]], wr=[R_stb[si]])

                        def rec_back(hd, b):
                            bs = slice(b * 128, (b + 1) * 128)
                            for c in range(4):
                                k.op("pe", lambda e, c=c: e.transpose(out=PT[:, c * 128:(c + 1) * 128],
                                                                      in_=og2[b % 2][:, c * 128:(c + 1) * 128], identity=ident[:]),
                                     rd=[R_og2[b % 2], R_ident], wr=[R_PT])
                            k.op("act", lambda e: e.activation(
                                out=ogT[:, hd * 4:(hd + 1) * 4, bs],
                                in_=PT[:, 0:512].rearrange("p (c t) -> p c t", c=4), func=AF.Copy),
                                rd=[R_PT], wr=[R_ogT])

                        for pc in make_pieces(0):
                            pc()
                        pend_back = None
                        for hd in range(4):
                            nxt = make_pieces(hd + 1) if hd < 3 else [None] * 8
                            for b in range(4):
                                rec_sd(hd, b)
                                if nxt[2 * b] is not None:
                                    nxt[2 * b]()
                                elif kv_pending:
                                    kv_pending.pop(0)()
                                    kv_pending.pop(0)()
                                rec_o(hd, b)
                                if nxt[2 * b + 1] is not None:
                                    nxt[2 * b + 1]()
                                elif kv_pending:
                                    kv_pending.pop(0)()
                                    kv_pending.pop(0)()
                                    if not kv_pending:
                                        load_dcs(t)
                                if pend_back is not None:
                                    rec_back(*pend_back)
                                pend_back = (hd, b)
                        rec_back(*pend_back)
                        stage(7)
                        if t + 1 < ntiles:
                            load_cs(t + 1)
                        gemm_T_acc(("wo", t), lambda kc, b: ogT[:, kc, b * 128:(b + 1) * 128], [R_ogT], [8, 8], resid_add)
                        stage(8)
                        for b in range(4):
                            rmsnorm_to_hT(xt[:, b, :], R_xt[b], gains[:, 1, :], R_gains,
                                          hT[:, :, b * 128:(b + 1) * 128], R_hT[b], tmps[b % 2], sq_on_dve=(b % 2 == 1))
                        for j in range(11):
                            wb, R_w = ws.next(("gu", t, j))
                            for fi in range(2):
                                gp = fi
                                Pg, R_Pg = (PB[4], R_PB[4]) if gp == 0 else (PB[6], R_PB[6])
                                Pu, R_Pu = (PB[5], R_PB[5]) if gp == 0 else (PB[3], R_PB[3])
                                sgf, R_sgf = sgf2[gp], R_sgf2[gp]
                                for P, R_P, c0 in ((Pg, R_Pg, fi * 128), (Pu, R_Pu, 256 + fi * 128)):
                                    for kc in range(8):
                                        k.op("pe", lambda e, kc=kc, P=P, c0=c0, wb=wb:
                                             e.matmul(P[:], lhsT=wb[:, kc, c0:c0 + 128], rhs=hT[:, kc, :],
                                                      start=(kc == 0), stop=(kc == 7)),
                                             rd=[R_w] + R_hT, wr=[R_P])
                                k.op("act", lambda e: e.activation(out=sgf[:], in_=Pg[:], func=AF.Silu),
                                     rd=[R_Pg], wr=[R_sgf])
                                fc = j * 2 + fi
                                k.op("dve", lambda e, fc=fc: e.tensor_tensor(out=actT[:, fc, :], in0=Pu[:], in1=sgf[:],
                                                                            op=ALU.mult),
                                     rd=[R_Pu, R_sgf], wr=[R_actT])
                        if t + 1 < ntiles:
                            norm0(t + 1)
                        gemm_T_acc(("dn", t), lambda kc, b: actT[:, kc, b * 128:(b + 1) * 128], [R_actT], [8, 8, 6], resid_add)
                        stage(9)
                        k.dma("sp", x1s[ts0:ts0 + 512, :].rearrange("(b p) d -> p b d", p=128), xt[:],
                              rd=R_xt, wr=[R_x1s[t]])
                        stage(10)
                        for b in range(4):
                            rmsnorm_to_hT(xt[:, b, :], R_xt[b], gains[:, 2, :], R_gains,
                                          hT[:, :, b * 128:(b + 1) * 128], R_hT[b], tmps[b % 2], sq_on_dve=(b % 2 == 1))

                        def make_kv_units(t=t, hTt=hT, R_hTt=R_hT):
                            stt = {"pend": None, "cnt": 0}
                            ts0k = t * 512

                            def unit(nci, b):
                                def f():
                                    if b == 0:
                                        stt["w"] = ws.next(("kv", t, nci))
                                    wb, R_w = stt["w"]
                                    u = stt["cnt"]
                                    stt["cnt"] += 1
                                    P, R_P = PB[u % 3], R_PB[u % 3]
                                    for kc in range(8):
                                        k.op("pe", lambda e, kc=kc: e.matmul(P[:], lhsT=hTt[:, kc, b * 128:(b + 1) * 128], rhs=wb[:, kc, :],
                                                                             start=(kc == 0), stop=(kc == 7)),
                                             rd=[R_w, R_hTt[b]], wr=[R_P])
                                    if nci < 2:
                                        pr = u % 2
                                        kkc, R_kkc = kk2[pr], R_kk2[pr]
                                        ra, R_ra = rt[2 * pr], R_rt[2 * pr]
                                        rb, R_rb = rt[2 * pr + 1], R_rt[2 * pr + 1]
                                        krc, R_krc = kr2[pr], R_kr2[pr]
                                        kst, R_kst = kst2[pr], R_kst2[pr]
                                        k.op("act", lambda e: e.activation(out=ra[:], in_=P[:], func=AF.Copy), rd=[R_P], wr=[R_ra])
                                        P3 = ra[:].rearrange("p (g d) -> p g d", d=64)
                                        kk3 = kkc[:].rearrange("p (g d) -> p g d", d=64)
                                        k.op("act", lambda e: e.activation(out=rb[:, 0:128], in_=dcsT[:, b, :], func=AF.Copy),
                                             rd=[R_dcsT], wr=[R_rb])
                                        cosb = rb[:, 0:64].rearrange("p (g d) -> p g d", d=8)
                                        sinb = rb[:, 64:128].rearrange("p (g d) -> p g d", d=8)
                                        k.op("act", lambda e: e.activation(out=kkc[:], in_=P[:], func=AF.Copy), rd=[R_P], wr=[R_kkc])
                                        for i, (lo, tb) in enumerate(((0, cosb), (8, sinb), (8, cosb), (0, sinb))):
                                            k.op("dve", lambda e, i=i, lo=lo, tb=tb: e.tensor_tensor(out=krc[i][:], in0=P3[:, :, lo:lo + 8], in1=tb,
                                                                                                      op=ALU.mult),
                                                 rd=[R_ra, R_rb], wr=[R_krc[i]])
                                        k.op("dve", lambda e: e.tensor_tensor(out=kk3[:, :, 0:8], in0=krc[0][:], in1=krc[1][:], op=ALU.subtract),
                                             rd=[R_krc[0], R_krc[1]], wr=[R_kkc])
                                        k.op("dve", lambda e: e.tensor_tensor(out=kk3[:, :, 8:16], in0=krc[2][:], in1=krc[3][:], op=ALU.add),
                                             rd=[R_krc[2], R_krc[3]], wr=[R_kkc])

                                        def back():
                                            for c in range(4):
                                                k.op("pe", lambda e, c=c: e.transpose(out=PT[:, c * 128:(c + 1) * 128],
                                                                                      in_=kkc[:, c * 128:(c + 1) * 128], identity=ident[:]),
                                                     rd=[R_kkc, R_ident], wr=[R_PT])
                                            k.op("act", lambda e: e.activation(out=kst[:], in_=PT[:, 0:512].rearrange("p (c t) -> p c t", c=4),
                                                                               func=AF.Copy), rd=[R_PT], wr=[R_kst])
                                            k.dma("sp", kTs[nci * 4:(nci + 1) * 4, :, ts0k + b * 128:ts0k + (b + 1) * 128].rearrange("h p s -> p h s"),
                                                  kst[:], rd=[R_kst], wr=[R_kTs[t]])
                                        if stt["pend"] is not None:
                                            stt["pend"]()
                                        stt["pend"] = back
                                    else:
                                        if stt["pend"] is not None:
                                            stt["pend"]()
                                            stt["pend"] = None
                                        pr = u % 2
                                        vstc, R_vstc = vst2[pr], R_vst2[pr]
                                        c0 = (nci - 2) * 512
                                        k.op("act", lambda e: e.activation(out=vstc[:], in_=P[:], func=AF.Copy), rd=[R_P], wr=[R_vstc])
                                        k.dma("sp", vs[ts0k + b * 128:ts0k + (b + 1) * 128, c0:c0 + 512], vstc[:], rd=[R_vstc], wr=[R_vs[t]])
                                return f
                            return [unit(nci, b) for nci in range(4) for b in range(4)]

                        kv_pending = make_kv_units()
                        if t == ntiles - 1:
                            for u_ in kv_pending:
                                u_()
                            kv_pending = []
              except _Stop:
                pass
              k.barrier()

        if "B" in phases:
            with ExitStack() as pes:
              try:
                hT2 = k.sb("hT2", [128, 8, 2048], BF16, pes); R_hT2 = [Reg() for _ in range(4)]
                ao = hT2[:].rearrange("p c t -> p (c t)").rearrange("p (j d) -> p j d", d=D)
                R_ao = Reg()
                bjunk = k.sb("bjunk", [128, D], BF16, pes); R_bjunk = Reg()
                bhn = k.sb("bhn", [128, D], BF16, pes); R_bhn = Reg()
                bsm = k.sb("bsm", [128, 24], F32, pes); R_bsm = [Reg() for _ in range(24)]
                bhn1 = k.sb("bhn1", [128, D], BF16, pes); R_bhn1 = Reg()
                bsmb = k.sb("bsmb", [128, 4], F32, pes)
                btmps = [(bjunk, R_bjunk, bsm[:, 0:1], R_bsm[0], bsm[:, 1:2], R_bsm[1], bsm[:, 2:3], R_bsm[2], bhn, R_bhn),
                         (bjunk, Reg(), bsmb[:, 0:1], Reg(), bsmb[:, 1:2], Reg(), bsmb[:, 2:3], Reg(), bhn1, R_bhn1)]
                xst = [k.sb("xst%d" % i, [128, 2, D], F32, pes) for i in range(2)]; R_xst = [Reg(), Reg()]
                xsel2 = [k.sb("xsel%d" % i, [128, D], F32, pes) for i in range(2)]; R_xsel2 = [Reg(), Reg()]
                gain1 = k.sb("gain1", [128, D], F32, pes); R_gain1 = Reg()
                lamt = k.sb("lamt", [128, 256], F32, pes); R_lamt = Reg()
                gsub = k.sb("gsub", [128, 128], F32, pes); R_gsub = Reg()
                amask = k.sb("amask", [128, 256], BF16, pes); R_amask = Reg()
                bcast_load(gain1[:], ln_mix[1:2, :], R_gain1)
                bcast_load(lamt[:], lam[0:1, :], R_lamt)
                bcast_load(gsub[:], subln[0:1, :], R_gsub)
                k.dma("pool", amask[:], amask_in[:, :, :].rearrange("p a q -> p (a q)"), wr=[R_amask])
                k.op("dve", lambda e: e.tensor_scalar(out=gsub[:], in0=gsub[:], scalar1=1.0 - LAMBDA_INIT, scalar2=None,
                                                      op0=ALU.mult), rd=[R_gsub], wr=[R_gsub])
                for i in range(2):
                    k.op("dve", lambda e, i=i: e.tensor_tensor(out=lamt[:, i * 128:i * 128 + 64], in0=lamt[:, i * 128:i * 128 + 64],
                                                               in1=lamt[:, i * 128 + 64:i * 128 + 128], op=ALU.mult),
                         rd=[R_lamt], wr=[R_lamt])
                    k.op("dve", lambda e, i=i: e.reduce_sum(out=bsm[:, 3 + i:4 + i], in_=lamt[:, i * 128:i * 128 + 64],
                                                            axis=mybir.AxisListType.X), rd=[R_lamt], wr=[R_bsm[3 + i]])
                    k.op("act", lambda e, i=i: e.activation(out=bsm[:, 5 + i:6 + i], in_=bsm[:, 3 + i:4 + i], func=AF.Exp),
                         rd=[R_bsm[3 + i]], wr=[R_bsm[5 + i]])
                k.op("dve", lambda e: e.scalar_tensor_tensor(out=bsm[:, 7:8], in0=bsm[:, 6:7], scalar=-LAMBDA_INIT, in1=bsm[:, 5:6],
                                                             op0=ALU.add, op1=ALU.subtract),
                     rd=[R_bsm[5], R_bsm[6]], wr=[R_bsm[7]])
                NEGLAM = bsm[:, 7:8]; R_NEGLAM = R_bsm[7]

                def load_own_block(j, dst_ap, R_dst):
                    xs, R_xs = xst[j % 2], R_xst[j % 2]
                    k.dma("sp", xs[:], x1s[2 * j * 128:(2 * j + 2) * 128, :].rearrange("(r p) d -> p r d", p=128),
                          rd=[R_x1s[(2 * j) // 4]], wr=[R_xs])
                    k.op("dve", lambda e: e.tensor_scalar(out=dst_ap, in0=xs[:, 0, :], scalar1=cst[:, 14:15], scalar2=None,
                                                          op0=ALU.mult), rd=[R_xs, R_cst], wr=[R_dst])
                    k.op("dve", lambda e: e.scalar_tensor_tensor(out=dst_ap, in0=xs[:, 1, :], scalar=cst[:, 15:16], in1=dst_ap,
                                                                 op0=ALU.mult, op1=ALU.add),
                         rd=[R_xs, R_cst, R_dst], wr=[R_dst])
                    k.dma("sp", xown[j * 128:(j + 1) * 128, :], dst_ap, rd=[R_dst], wr=[R_xown[j // 4]])

                with ExitStack() as pes2:
                    specs = []
                    for g in range(4):
                        for nh in range(2):
                            specs.append((("wq", g, nh), [(0, 0, w_q[:, nh * 512:(nh + 1) * 512], 8, 512)]))
                    ws = WStream(k, specs, pes2, name="wbB")
                    qTa = k.sb("qTa", [128, 8, 2048], BF16, pes2); R_qTa = [Reg() for _ in range(16)]
                    dq = k.sb("dq", [128, 4, 128], F32, pes2); R_dq = Reg()
                    brt = [k.sb("brt%d" % i, [128, 512], F32, pes2) for i in range(4)]; R_brt = [Reg() for _ in range(4)]
                    qq2 = [k.sb("qq%d" % i, [128, 512], BF16, pes2) for i in range(2)]; R_qq2 = [Reg(), Reg()]
                    bkr2 = [[k.sb("bkr%d_%d" % (jj, i), [128, 8, 8], F32, pes2) for i in range(4)] for jj in range(2)]
                    R_bkr2 = [[Reg() for _ in range(4)] for _ in range(2)]
                    KT = [k.sb("KT%d" % i, [128, S], BF16, pes2) for i in range(2)]; R_KT = [Reg(), Reg()]
                    VX = [k.sb("VX%d" % i, [128, 32, 132], BF16, pes2) for i in range(2)]; R_VX = [Reg(), Reg()]
                    PTs = [[k.sb("PTs%d%d" % (c, i), [128, 32 * 128], BF16, pes2) for i in range(2)] for c in range(2)]
                    R_PTs = [[Reg(), Reg()], [Reg(), Reg()]]
                    tcomb2 = [k.sb("tcomb%d" % i, [128, 128], F32, pes2) for i in range(2)]; R_tcomb2 = [Reg(), Reg()]
                    osb2 = [k.sb("osb%d" % i, [128, 128], F32, pes2) for i in range(2)]; R_osb2 = [Reg(), Reg()]
                    bsm2 = [k.sb("bsm2_%d" % i, [128, 8], F32, pes2) for i in range(2)]
                    R_bsm2 = [[Reg() for _ in range(8)] for _ in range(2)]
                    ojunk = k.sb("ojunk", [128, 128], BF16, pes2); R_ojunk = Reg()
                    for i in range(2):
                        k.op("pool", lambda e, i=i: e.memset(VX[i][:, :, 128:129], 1.0), wr=[R_VX[i]])
                    pend = None
                    qc = 0

                    def prep_front(j):
                        load_own_block(j, xsel2[j % 2][:], R_xsel2[j % 2])
                        return rmsnorm_to_hT(xsel2[j % 2][:], R_xsel2[j % 2], gain1[:], R_gain1, hT2[:, :, j * 128:(j + 1) * 128],
                                             R_hT2[j // 4], btmps[j % 2], defer=True)
                    for b in range(4):
                        prep_front(b)()
                    for g in range(4):
                        k.dma("sp", dq[:], dcs_own[g * 512:(g + 1) * 512, :].rearrange("(b p) d -> p b d", p=128), wr=[R_dq])
                        backs = {}
                        kitem = 0
                        for nh in range(2):
                            wb, R_w = ws.next(("wq", g, nh))
                            for b in range(4):
                                j = 4 * g + b
                                P, R_P = PB[b], R_PB[b]
                                for kc in range(8):
                                    k.op("pe", lambda e, kc=kc, P=P, wb=wb, j=j:
                                         e.matmul(P[:], lhsT=hT2[:, kc, j * 128:(j + 1) * 128], rhs=wb[:, kc, :],
                                                  start=(kc == 0), stop=(kc == 7)),
                                         rd=[R_w, R_hT2[g]], wr=[R_P])
                                pr = qc % 2
                                qc += 1
                                ra, R_ra = brt[2 * pr], R_brt[2 * pr]
                                rb, R_rb = brt[2 * pr + 1], R_brt[2 * pr + 1]
                                qqc, R_qqc = qq2[pr], R_qq2[pr]
                                krc, R_krc = bkr2[pr], R_bkr2[pr]
                                k.op("act", lambda e, P=P: e.activation(out=ra[:], in_=P[:], func=AF.Copy, scale=0.125),
                                     rd=[R_P], wr=[R_ra])
                                k.op("act", lambda e, b=b: e.activation(out=rb[:, 0:128], in_=dq[:, b, :], func=AF.Copy),
                                     rd=[R_dq], wr=[R_rb])
                                k.op("act", lambda e, P=P: e.activation(out=qqc[:], in_=P[:], func=AF.Copy, scale=0.125),
                                     rd=[R_P], wr=[R_qqc])
                                P3 = ra[:].rearrange("p (g d) -> p g d", d=64)
                                qq3 = qqc[:].rearrange("p (g d) -> p g d", d=64)
                                cosb = rb[:, 0:64].rearrange("p (g d) -> p g d", d=8)
                                sinb = rb[:, 64:128].rearrange("p (g d) -> p g d", d=8)
                                for i, (lo, tb) in enumerate(((0, cosb), (8, sinb), (8, cosb), (0, sinb))):
                                    k.op("dve", lambda e, i=i, lo=lo, tb=tb: e.tensor_tensor(out=krc[i][:], in0=P3[:, :, lo:lo + 8], in1=tb,
                                                                                              op=ALU.mult),
                                         rd=[R_ra, R_rb], wr=[R_krc[i]])
                                k.op("dve", lambda e: e.tensor_tensor(out=qq3[:, :, 0:8], in0=krc[0][:], in1=krc[1][:], op=ALU.subtract),
                                     rd=[R_krc[0], R_krc[1]], wr=[R_qqc])
                                k.op("dve", lambda e: e.tensor_tensor(out=qq3[:, :, 8:16], in0=krc[2][:], in1=krc[3][:], op=ALU.add),
                                     rd=[R_krc[2], R_krc[3]], wr=[R_qqc])

                                def backq(nh=nh, j=j, qqc=qqc, R_qqc=R_qqc):
                                    for c in range(4):
                                        k.op("pe", lambda e, c=c: e.transpose(out=PT[:, c * 128:(c + 1) * 128],
                                                                              in_=qqc[:, c * 128:(c + 1) * 128], identity=ident[:]),
                                             rd=[R_qqc, R_ident], wr=[R_PT])
                                    k.op("act", lambda e: e.activation(
                                        out=qTa[:, nh * 4:(nh + 1) * 4, j * 128:(j + 1) * 128],
                                        in_=PT[:, 0:512].rearrange("p (c t) -> p c t", c=4), func=AF.Copy),
                                        rd=[R_PT], wr=[R_qTa[j]])
                                if pend is not None:
                                    pend()
                                pend = backq
                                if g < 3:
                                    bb = kitem // 2
                                    if kitem % 2 == 0:
                                        backs[bb] = prep_front(4 * (g + 1) + bb)
                                    else:
                                        backs[bb]()
                                kitem += 1
                    pend()
                    def load_head(h):
                        i = h % 2
                        k.dma("sp", KT[i][:], kTs[h, :, :], rd=R_kTs, wr=[R_KT[i]])
                        k.dma("sp", VX[i][:, :, 0:128], vs[:, h * 128:(h + 1) * 128].rearrange("(n p) d -> p n d", p=128),
                              rd=R_vs, wr=[R_VX[i]])
                    sb_cnt = [0]

                    def att_front(h, j):
                        hi = h % 2
                        nk = 2 * j + 2
                        jp = j % 2
                        for c in range(2):
                            pts, R_pts = PTs[c][jp], R_PTs[c][jp]
                            for g0 in range(0, nk, 4):
                                ng = min(4, nk - g0)
                                bk, R_bk = PB[sb_cnt[0] % 3], R_PB[sb_cnt[0] % 3]
                                sb_cnt[0] += 1
                                for i in range(ng):
                                    kb = g0 + i
                                    k.op("pe", lambda e, i=i, kb=kb: e.matmul(bk[:, i * 128:(i + 1) * 128],
                                                                             lhsT=KT[hi][c * 64:(c + 1) * 64, kb * 128:(kb + 1) * 128],
                                                                             rhs=qTa[c * 64:(c + 1) * 64, h, j * 128:(j + 1) * 128],
                                                                             start=True, stop=True),
                                         rd=[R_KT[hi], R_qTa[j]], wr=[R_bk])
                                k.op("act", lambda e: e.activation(out=pts[:, g0 * 128:(g0 + ng) * 128], in_=bk[:, 0:ng * 128], func=AF.Exp),
                                     rd=[R_bk], wr=[R_pts])
                            k.op("pool", lambda e: e.tensor_tensor(out=pts[:, 2 * j * 128:(2 * j + 2) * 128],
                                                                   in0=pts[:, 2 * j * 128:(2 * j + 2) * 128],
                                                                   in1=amask[:], op=ALU.mult),
                                 rd=[R_pts, R_amask], wr=[R_pts])

                    def att_back(h, j):
                        hi = h % 2
                        nk = 2 * j + 2
                        jp = j % 2
                        PO = [PB[3 + 2 * jp], PB[4 + 2 * jp]]
                        R_PO = [R_PB[3 + 2 * jp], R_PB[4 + 2 * jp]]
                        for c in range(2):
                            pts, R_pts = PTs[c][jp], R_PTs[c][jp]
                            for kb in range(nk):
                                k.op("pe", lambda e, kb=kb: e.matmul(PO[c][:, 0:129], lhsT=pts[:, kb * 128:(kb + 1) * 128],
                                                                     rhs=VX[hi][:, kb, 0:129], start=(kb == 0), stop=(kb == nk - 1)),
                                     rd=[R_pts, R_VX[hi]], wr=[R_PO[c]])
                        sm_ = bsm2[jp]
                        R_s = R_bsm2[jp]
                        k.op("dve", lambda e: e.reciprocal(out=sm_[:, 0:1], in_=PO[0][:, 128:129]), rd=[R_PO[0]], wr=[R_s[0]])
                        k.op("dve", lambda e: e.reciprocal(out=sm_[:, 1:2], in_=PO[1][:, 128:129]), rd=[R_PO[1]], wr=[R_s[1]])
                        k.op("dve", lambda e: e.tensor_tensor(out=sm_[:, 2:3], in0=sm_[:, 1:2], in1=NEGLAM, op=ALU.mult),
                             rd=[R_s[1], R_NEGLAM], wr=[R_s[2]])
                        k.op("dve", lambda e: e.tensor_scalar(out=tcomb2[jp][:], in0=PO[1][:, 0:128], scalar1=sm_[:, 2:3], scalar2=None,
                                                              op0=ALU.mult), rd=[R_PO[1], R_s[2]], wr=[R_tcomb2[jp]])
                        k.op("dve", lambda e: e.scalar_tensor_tensor(out=osb2[jp][:], in0=PO[0][:, 0:128], scalar=sm_[:, 0:1],
                                                                     in1=tcomb2[jp][:], op0=ALU.mult, op1=ALU.add),
                             rd=[R_PO[0], R_s[0], R_tcomb2[jp]], wr=[R_osb2[jp]])
                        k.op("act", lambda e: e.activation(out=ojunk[:], in_=osb2[jp][:], func=AF.Square, accum_out=sm_[:, 3:4]),
                             rd=[R_osb2[jp]], wr=[R_s[3]])
                        k.op("dve", lambda e: e.tensor_scalar(out=sm_[:, 4:5], in0=sm_[:, 3:4], scalar1=1.0 / 128, scalar2=EPS,
                                                              op0=ALU.mult, op1=ALU.add), rd=[R_s[3]], wr=[R_s[4]])
                        k.op("pool", lambda e: e.tensor_tensor(out=sm_[:, 5:6], in0=sm_[:, 4:5], in1=cst[:, 12:13], op=ALU.pow),
                             rd=[R_s[4], R_cst], wr=[R_s[5]])
                        k.op("dve", lambda e: e.scalar_tensor_tensor(out=ao[:, j, h * 128:(h + 1) * 128], in0=osb2[jp][:],
                                                                     scalar=sm_[:, 5:6], in1=gsub[:], op0=ALU.mult, op1=ALU.mult),
                             rd=[R_osb2[jp], R_s[5], R_gsub], wr=[R_ao] + R_hT2)

                    load_head(0)
                    items = [(h, j) for h in range(8) for j in range(16)]
                    for idx, (h, j) in enumerate(items):
                        att_front(h, j)
                        if idx > 0:
                            att_back(*items[idx - 1])
                        if j == 0 and h + 1 < 8:
                            load_head(h + 1)
                    att_back(*items[-1])
                    k.barrier()
                with ExitStack() as pes3:
                    specs = []
                    for g in range(4):
                        for nh in range(2):
                            specs.append((("wdo", g, nh), [(0, 0, w_do[:, nh * 512:(nh + 1) * 512], 8, 512)]))
                    ws = WStream(k, specs, pes3, name="wbB3")
                    xo2 = [k.sb("xo%d" % i, [128, 4, D], F32, pes3) for i in range(2)]
                    R_xo2 = [[Reg() for _ in range(4)] for _ in range(2)]
                    aoT2 = [k.sb("aoT%d" % i, [128, 8, 512], BF16, pes3) for i in range(2)]; R_aoT2 = [Reg(), Reg()]

                    def b3_prep(g):
                        xo, R_xo, aoT, R_aoT = xo2[g % 2], R_xo2[g % 2], aoT2[g % 2], R_aoT2[g % 2]
                        k.dma("sp", xo[:], xown[g * 512:(g + 1) * 512, :].rearrange("(b p) d -> p b d", p=128),
                              rd=[R_xown[g]], wr=R_xo)
                        for b in range(4):
                            j = 4 * g + b
                            for c in range(8):
                                k.op("pe", lambda e, c=c, j=j: e.transpose(out=PT[:, c * 128:(c + 1) * 128],
                                                                           in_=ao[:, j, c * 128:(c + 1) * 128], identity=ident[:]),
                                     rd=[R_ao, R_ident], wr=[R_PT])
                            k.op("act", lambda e, b=b: e.activation(out=aoT[:, :, b * 128:(b + 1) * 128],
                                                                    in_=PT[:].rearrange("p (c t) -> p c t", c=8), func=AF.Copy),
                                 rd=[R_PT], wr=[R_aoT])

                    def b3_gemm(g):
                        xo, R_xo, aoT, R_aoT = xo2[g % 2], R_xo2[g % 2], aoT2[g % 2], R_aoT2[g % 2]
                        for nh in range(2):
                            wb, R_w = ws.next(("wdo", g, nh))
                            for b in range(4):
                                P, R_P = PB[b], R_PB[b]
                                for kc in range(8):
                                    k.op("pe", lambda e, kc=kc, P=P, wb=wb, b=b:
                                         e.matmul(P[:], lhsT=aoT[:, kc, b * 128:(b + 1) * 128], rhs=wb[:, kc, :],
                                                  start=(kc == 0), stop=(kc == 7)),
                                         rd=[R_w, R_aoT], wr=[R_P])
                                k.op("dve", lambda e, b=b, nh=nh, P=P: e.tensor_tensor(out=xo[:, b, nh * 512:(nh + 1) * 512],
                                                                                      in0=xo[:, b, nh * 512:(nh + 1) * 512], in1=P[:],
                                                                                      op=ALU.add),
                                     rd=[R_P, R_xo[b]], wr=[R_xo[b]])
                        k.dma("sp", xbs[g * 512:(g + 1) * 512, :].rearrange("(b p) d -> p b d", p=128), xo[:], rd=R_xo, wr=[R_xbs[g]])

                    b3_prep(0)
                    for g in range(4):
                        if g + 1 < 4:
                            b3_prep(g + 1)
                        b3_gemm(g)
              except _Stop:
                pass
              k.barrier()

        if "C" in phases:
            with ExitStack() as pes:
              try:
                xres = k.sb("xres", [128, 16, D], F32, pes); R_xres = [[Reg(), Reg()] for _ in range(16)]
                hT3 = k.sb("hT3", [128, 8, 2048], BF16, pes); R_hT3 = [Reg() for _ in range(4)]
                gates = k.sb("gates", [128, 16, 8], F32, pes); R_gates = Reg()
                rw = k.sb("rw", [128, 8, 8], BF16, pes); R_rw = Reg()
                actg = [k.sb("actg%d" % i, [128, 4, 2048], BF16, pes) for i in range(2)]; R_actg = [Reg(), Reg()]
                sgc = [k.sb("sgc%d" % i, [128, 512], BF16, pes) for i in range(2)]; R_sgc = [Reg(), Reg()]
                gain2 = k.sb("gain2", [128, 2, D], F32, pes); R_gain2 = Reg()
                cjunk = k.sb("cjunk", [128, D], BF16, pes); R_cjunk = Reg()
                chn = k.sb("chn", [128, D], BF16, pes); R_chn = Reg()
                csm = k.sb("csm", [128, 24], F32, pes); R_csm = [Reg() for _ in range(24)]
                chn1 = k.sb("chn1", [128, D], BF16, pes)
                csmb = k.sb("csmb", [128, 4], F32, pes)
                ctmps = [(cjunk, R_cjunk, csm[:, 0:1], R_csm[0], csm[:, 1:2], R_csm[1], csm[:, 2:3], R_csm[2], chn, R_chn),
                         (cjunk, Reg(), csmb[:, 0:1], Reg(), csmb[:, 1:2], Reg(), csmb[:, 2:3], Reg(), chn1, Reg())]
                lg = k.sb("lg", [128, 8], F32, pes); R_lg = Reg()
                ge = [k.sb("ge%d" % i, [128, 8], F32, pes) for i in range(4)]; R_ge = [Reg() for _ in range(4)]
                ost = [k.sb("ost%d" % i, [128, D], F32, pes) for i in range(2)]; R_ost = [Reg(), Reg()]
                tmpc = [k.sb("tmpc%d" % i, [128, 512], F32, pes) for i in range(2)]; R_tmpc = [Reg(), Reg()]
                bcast_load(gain2[:, 0, :], ln_ffn[1:2, :], R_gain2)
                bcast_load(gain2[:, 1, :], final_norm[0:1, :], R_gain2)
                k.dma("pool", rw[:], router.rearrange("(kc p) n -> p kc n", p=128), wr=[R_rw])
                specs = []
                for ex in range(NEXP):
                    for fg in range(7):
                        for i in range(2):
                            f0 = (fg * 4 + 2 * i) * 128
                            specs.append((("gu", ex, fg, i), [(0, 0, moe_gu[ex, :, f0:f0 + 256], 8, 256),
                                                              (0, 256, moe_gu[ex, :, EXD + f0:EXD + f0 + 256], 8, 256)]))
                        specs.append((("dn", ex, fg), [(0, 0, moe_dn[ex, fg * 512:(fg + 1) * 512, 0:512], 4, 512),
                                                       (4, 0, moe_dn[ex, fg * 512:(fg + 1) * 512, 512:1024], 4, 512)]))
                ws = WStream(k, specs, pes, nbuf=5, pf=2, name="wbC")
                for g in range(4):
                    k.dma("sp", xres[:, g * 4:(g + 1) * 4, :], xbs[g * 512:(g + 1) * 512, :].rearrange("(b p) d -> p b d", p=128),
                          rd=[R_xbs[g]], wr=[r for b in range(4) for r in R_xres[g * 4 + b]])
                for j in range(16):
                    rmsnorm_to_hT(xres[:, j, :], R_xres[j][0], gain2[:, 0, :], R_gain2, hT3[:, :, j * 128:(j + 1) * 128],
                                  R_hT3[j // 4], ctmps[j % 2], sq_on_dve=(j % 2 == 1))
                    Pl, R_Pl = PB[0], R_PB[0]
                    for kc in range(8):
                        k.op("pe", lambda e, kc=kc, j=j: e.matmul(Pl[:, 0:8], lhsT=hT3[:, kc, j * 128:(j + 1) * 128], rhs=rw[:, kc, :],
                                                                  start=(kc == 0), stop=(kc == 7)),
                             rd=[R_hT3[j // 4], R_rw], wr=[R_Pl])
                    k.op("act", lambda e: e.activation(out=lg[:], in_=Pl[:, 0:8], func=AF.Copy), rd=[R_Pl], wr=[R_lg])
                    k.op("dve", lambda e: e.reduce_max(out=csm[:, 3:4], in_=lg[:], axis=mybir.AxisListType.X), rd=[R_lg], wr=[R_csm[3]])
                    k.op("dve", lambda e: e.tensor_scalar(out=ge[0][:], in0=lg[:], scalar1=csm[:, 3:4], scalar2=None, op0=ALU.is_equal),
                         rd=[R_lg, R_csm[3]], wr=[R_ge[0]])
                    k.op("dve", lambda e: e.scalar_tensor_tensor(out=ge[1][:], in0=ge[0][:], scalar=-1e30, in1=lg[:],
                                                                 op0=ALU.mult, op1=ALU.add), rd=[R_ge[0], R_lg], wr=[R_ge[1]])
                    k.op("dve", lambda e: e.reduce_max(out=csm[:, 4:5], in_=ge[1][:], axis=mybir.AxisListType.X), rd=[R_ge[1]], wr=[R_csm[4]])
                    k.op("dve", lambda e: e.tensor_scalar(out=ge[2][:], in0=ge[1][:], scalar1=csm[:, 4:5], scalar2=None, op0=ALU.is_equal),
                         rd=[R_ge[1], R_csm[4]], wr=[R_ge[2]])
                    k.op("dve", lambda e: e.tensor_tensor(out=csm[:, 5:6], in0=csm[:, 4:5], in1=csm[:, 3:4], op=ALU.subtract),
                         rd=[R_csm[3], R_csm[4]], wr=[R_csm[5]])
                    k.op("act", lambda e: e.activation(out=csm[:, 6:7], in_=csm[:, 5:6], func=AF.Sigmoid), rd=[R_csm[5]], wr=[R_csm[6]])
                    k.op("dve", lambda e: e.tensor_scalar(out=csm[:, 7:8], in0=csm[:, 6:7], scalar1=-1.0, scalar2=1.0,
                                                          op0=ALU.mult, op1=ALU.add), rd=[R_csm[6]], wr=[R_csm[7]])
                    k.op("dve", lambda e: e.tensor_scalar(out=ge[3][:], in0=ge[0][:], scalar1=csm[:, 7:8], scalar2=None, op0=ALU.mult),
                         rd=[R_ge[0], R_csm[7]], wr=[R_ge[3]])
                    k.op("dve", lambda e, j=j: e.scalar_tensor_tensor(out=gates[:, j, :], in0=ge[2][:], scalar=csm[:, 6:7], in1=ge[3][:],
                                                                     op0=ALU.mult, op1=ALU.add),
                         rd=[R_ge[2], R_ge[3], R_csm[6]], wr=[R_gates])
                stage(30)
                gcnt = 0
                dcnt = [0]
                pending = []

                def make_down_units(ex, ab, R_ab, wd, R_wd):
                    units = []
                    for b in range(16):
                        for nh in range(2):
                            def unit(b=b, nh=nh):
                                P, R_P = PB[dcnt[0] % 3], R_PB[dcnt[0] % 3]
                                dcnt[0] += 1
                                for fcl in range(4):
                                    k.op("pe", lambda e, fcl=fcl: e.matmul(P[:], lhsT=ab[:, fcl, b * 128:(b + 1) * 128],
                                                                           rhs=wd[:, nh * 4 + fcl, :], start=(fcl == 0), stop=(fcl == 3)),
                                         rd=[R_wd, R_ab], wr=[R_P])
                                k.op("dve", lambda e: e.scalar_tensor_tensor(
                                    out=xres[:, b, nh * 512:(nh + 1) * 512], in0=P[:], scalar=gates[:, b, ex:ex + 1],
                                    in1=xres[:, b, nh * 512:(nh + 1) * 512], op0=ALU.mult, op1=ALU.add),
                                    rd=[R_P, R_gates, R_xres[b][nh]], wr=[R_xres[b][nh]])
                            units.append(unit)
                    return units

                for ex in range(NEXP):
                    for fg in range(7):
                        ab, R_ab = actg[(ex * 7 + fg) % 2], R_actg[(ex * 7 + fg) % 2]
                        for i in range(2):
                            wb, R_w = ws.next(("gu", ex, fg, i))
                            for fi in range(2):
                                fcl = 2 * i + fi
                                for q in range(4):
                                    pp = gcnt % 2
                                    gcnt += 1
                                    Pg, R_Pg = PB[3 + 2 * pp], R_PB[3 + 2 * pp]
                                    Pu, R_Pu = PB[4 + 2 * pp], R_PB[4 + 2 * pp]
                                    for P, R_P, c0 in ((Pg, R_Pg, fi * 128), (Pu, R_Pu, 256 + fi * 128)):
                                        for kc in range(8):
                                            k.op("pe", lambda e, kc=kc, P=P, c0=c0, wb=wb, q=q:
                                                 e.matmul(P[:], lhsT=wb[:, kc, c0:c0 + 128], rhs=hT3[:, kc, q * 512:(q + 1) * 512],
                                                          start=(kc == 0), stop=(kc == 7)),
                                                 rd=[R_w, R_hT3[q]], wr=[R_P])
                                    k.op("act", lambda e, pp=pp, Pg=Pg: e.activation(out=sgc[pp][:], in_=Pg[:], func=AF.Silu),
                                         rd=[R_Pg], wr=[R_sgc[pp]])
                                    k.op("dve", lambda e, pp=pp, Pu=Pu, fcl=fcl, q=q, ab=ab:
                                         e.tensor_tensor(out=ab[:, fcl, q * 512:(q + 1) * 512], in0=Pu[:], in1=sgc[pp][:], op=ALU.mult),
                                         rd=[R_Pu, R_sgc[pp]], wr=[R_ab])
                                    for _ in range(2):
                                        if pending:
                                            pending.pop(0)()
                        wd, R_wd = ws.next(("dn", ex, fg))
                        assert not pending
                        pending = make_down_units(ex, ab, R_ab, wd, R_wd)
                    stage(31 + ex)
                def final_norm(j):
                    o_t, R_o = ost[j % 2], R_ost[j % 2]
                    k.op("act", lambda e: e.activation(out=cjunk[:], in_=xres[:, j, :], func=AF.Square, accum_out=csm[:, 8:9]),
                         rd=R_xres[j], wr=[R_cjunk, R_csm[8]])
                    k.op("dve", lambda e: e.tensor_scalar(out=csm[:, 9:10], in0=csm[:, 8:9], scalar1=1.0 / D, scalar2=EPS,
                                                          op0=ALU.mult, op1=ALU.add), rd=[R_csm[8]], wr=[R_csm[9]])
                    k.op("pool", lambda e: e.tensor_tensor(out=csm[:, 10:11], in0=csm[:, 9:10], in1=cst[:, 12:13], op=ALU.pow),
                         rd=[R_csm[9], R_cst], wr=[R_csm[10]])
                    k.op("dve", lambda e: e.scalar_tensor_tensor(out=o_t[:], in0=xres[:, j, :], scalar=csm[:, 10:11],
                                                                 in1=gain2[:, 1, :], op0=ALU.mult, op1=ALU.mult),
                         rd=R_xres[j] + [R_csm[10], R_gain2], wr=[R_o])
                    k.dma("sp", out[j * 128:(j + 1) * 128, :], o_t[:], rd=[R_o], wr=[R_out])

                assert len(pending) == 32
                for j in range(16):
                    pending.pop(0)()
                    pending.pop(0)()
                    if j >= 1:
                        final_norm(j - 1)
                final_norm(15)
              except _Stop:
                pass
              k.barrier()

        outs = [R_out] if R_out.dsem is not None else []
        if debug:
            outs += [r for r in (R_x1s + R_kTs + R_vs + R_xbs) if r.dsem is not None]
        k.finish(outs)
        print("ops", k.nops, "waits", k.nwaits, "sig", k.nsig, "dsems", k.nsem)
    _DECL[id(nc)] = list(declared)
    return nc


_DECL = {}


def make_in_maps(inputs):
    HC = host_consts()
    x = np.asarray(inputs["x"], np.float32)
    f = lambda a: np.ascontiguousarray(np.asarray(a, np.float32))
    shared = {
        "ln_mix": f(inputs["ln_mix"]), "ln_ffn": f(inputs["ln_ffn"]),
        "ret_w_in": f(inputs["ret_w_in"][0]), "ret_w_o": f(inputs["ret_w_o"][0]),
        "kv_norm": f(inputs["kv_norm"]).reshape(1, D), "w_kv": f(inputs["w_kv"]),
        "diff_w_q": f(inputs["diff_w_q"][0]),
        "lam": np.concatenate([f(inputs["lam_q1"][0]), f(inputs["lam_k1"][0]),
                               f(inputs["lam_q2"][0]), f(inputs["lam_k2"][0])]).reshape(1, 256),
        "diff_subln": f(inputs["diff_subln"]).reshape(1, 128), "diff_w_o": f(inputs["diff_w_o"][0]),
        "ffn_w_gu": f(inputs["ffn_w_gu"][0]), "ffn_w_down": f(inputs["ffn_w_down"][0]),
        "moe_router": f(inputs["moe_router"][0]), "moe_w_gu": f(inputs["moe_w_gu"][0]),
        "moe_w_down": f(inputs["moe_w_down"][0]), "final_norm": f(inputs["final_norm"]).reshape(1, D),
        "rcos": HC["rcos"], "rsin": HC["rsin"], "dcs": HC["dcs"], "maskT": HC["maskT"], "ident": HC["ident"],
    }
    maps = []
    kq = np.arange(128)
    diag = ((kq[:, None] // 64) <= (kq[None, :] // 64)).astype(np.float32)
    for c in range(NCORE):
        b, h = c // 2, c % 2
        m = dict(shared)
        m["x"] = np.ascontiguousarray(x[b])
        cst = HC["cst"].copy()
        cst[:, 14] = 1.0 if h == 0 else 0.0
        cst[:, 15] = 1.0 if h == 1 else 0.0
        m["cst"] = cst
        own = np.concatenate([np.arange((2 * j + h) * 128, (2 * j + h + 1) * 128) for j in range(16)])
        m["dcs_own"] = np.ascontiguousarray(HC["dcs"][own])
        am = np.zeros((128, 2, 128), np.float32)
        if h == 0:
            am[:, 0, :] = diag
        else:
            am[:, 0, :] = 1.0
            am[:, 1, :] = diag
        m["amask"] = am
        maps.append(m)
    return maps


_NC_CACHE = {}


def kernel(**inputs):
    if "nc" not in _NC_CACHE:
        _NC_CACHE["nc"] = build_program()
    nc = _NC_CACHE["nc"]
    maps = make_in_maps(inputs)
    res = run_bass_kernel_spmd(nc, maps, core_ids=list(range(NCORE)))
    out = np.zeros((4, S, D), np.float32)
    for c in range(NCORE):
        b, h = c // 2, c % 2
        o = np.asarray(res.results[c]["out"]).reshape(16, 128, D)
        for j in range(16):
            out[b, (2 * j + h) * 128:(2 * j + h + 1) * 128] = o[j]
    return out
```

```python
import bisect
import math
from contextlib import ExitStack

import numpy as np
import concourse.bass as bass
import concourse.mybir as mybir
from concourse.bass_utils import run_bass_kernel_spmd

F32 = mybir.dt.float32
BF16 = mybir.dt.bfloat16
AF = mybir.ActivationFunctionType
ALU = mybir.AluOpType

ENGS = ("pe", "act", "dve", "pool", "sp")
D = 1024
S = 4096
NCORE = 8
EPS = 1e-6
FFN = 2816
EXD = 3584
NEXP = 8
LAMBDA_INIT = 0.8 - 0.6 * math.exp(-0.3 * 1)


class Reg:
    __slots__ = ("name", "w", "r", "dsem", "dcnt")

    def __init__(self, name=""):
        self.name = name
        self.w = None
        self.r = []
        self.dsem = None
        self.dcnt = 0


class KB:
    def __init__(self, nc, es):
        self.nc = nc
        self.es = es
        self.eng = {"pe": nc.tensor, "act": nc.scalar, "dve": nc.vector,
                    "pool": nc.gpsimd, "sp": nc.sync}
        self.sem = {e: es.enter_context(nc.semaphore("c_" + e)) for e in ENGS}
        self.nsig = {e: 0 for e in ENGS}
        self.seq = {e: 0 for e in ENGS}
        self.last = {e: None for e in ENGS}
        self.sig_seq = {e: [] for e in ENGS}
        self.sig_snap = {e: [] for e in ENGS}
        self.seen = {e: {e2: 0 for e2 in ENGS} for e in ENGS}
        self.seen_d = {e: {} for e in ENGS}
        self.snap_last = {e: None for e in ENGS}
        self.dma_regs = []
        self.nwaits = 0
        self.nsem = 0
        self.nops = {e: 0 for e in ENGS}

    def sb(self, name, shape, dt, es=None):
        return (es or self.es).enter_context(self.nc.sbuf_tensor("s_" + name, list(shape), dt))

    def ps(self, name, shape, dt, es=None):
        return (es or self.es).enter_context(self.nc.psum_tensor("p_" + name, list(shape), dt))

    def dram(self, name, shape, dt, kind="Internal"):
        return self.nc.dram_tensor(name, list(shape), dt, kind=kind).ap()

    def _token_for(self, e, seq):
        ss = self.sig_seq[e]
        i = bisect.bisect_left(ss, seq)
        if i < len(ss):
            return i + 1
        ins = self.last[e]
        assert ins is not None and self.seq[e] >= seq, (e, seq, self.seq[e])
        ins.then_inc(self.sem[e], 1)
        self.nsig[e] += 1
        ss.append(self.seq[e])
        self.sig_snap[e].append(self.snap_last[e])
        self.last[e] = None
        return self.nsig[e]

    def _wait(self, e, tok):
        if tok is None:
            return
        if tok[0] == "c":
            _, e2, seq = tok
            if e2 == e and e == "pe":
                return
            ss = self.sig_seq[e2]
            known = self.seen[e][e2]
            if known > 0 and ss[known - 1] >= seq:
                return
            n = self._token_for(e2, seq)
            if self.seen[e][e2] >= n:
                return
            self.eng[e].wait_ge(self.sem[e2], n)
            self.nwaits += 1
            self.seen[e][e2] = n
            snap = self.sig_snap[e2][n - 1]
            if snap is not None:
                for e3, v in snap.items():
                    if v > self.seen[e][e3]:
                        self.seen[e][e3] = v
        else:
            _, sem, cnt, sid = tok
            if self.seen_d[e].get(sid, 0) >= cnt:
                return
            self.eng[e].wait_ge(sem, cnt)
            self.nwaits += 1
            self.seen_d[e][sid] = cnt

    def _deps(self, e, rd, wr):
        for r in rd:
            self._wait(e, r.w)
        for r in wr:
            self._wait(e, r.w)
            for t in r.r:
                self._wait(e, t)

    def op(self, e, fn, rd=(), wr=()):
        self._deps(e, rd, wr)
        ins = fn(self.eng[e])
        self.seq[e] += 1
        self.nops[e] += 1
        self.last[e] = ins
        self.snap_last[e] = dict(self.seen[e])
        tok = ("c", e, self.seq[e])
        for r in rd:
            r.r.append(tok)
        for r in wr:
            r.w = tok
            r.r = []
        return ins

    def dma(self, q, out, in_, rd=(), wr=(), **kw):
        self._deps(q, rd, wr)
        d = wr[0]
        if d.dsem is None:
            d.dsem = self.es.enter_context(self.nc.semaphore("d%d" % self.nsem))
            self.nsem += 1
            self.dma_regs.append(d)
        ins = self.eng[q].dma_start(out=out, in_=in_, **kw)
        ins.then_inc(d.dsem, 16)
        d.dcnt += 16
        tok = ("d", d.dsem, d.dcnt, id(d))
        for r in rd:
            r.r.append(tok)
        for r in wr:
            r.w = tok
            r.r = []
        return ins

    def barrier(self):
        sp = "sp"
        for d in self.dma_regs:
            if d.dcnt:
                self._wait(sp, ("d", d.dsem, d.dcnt, id(d)))
        for e in ENGS:
            if e != sp and self.seq[e] > 0:
                self._wait(sp, ("c", e, self.seq[e]))
        self.eng[sp].sem_inc(self.sem[sp], 1)
        self.nsig[sp] += 1
        self.seq[sp] += 1
        self.sig_seq[sp].append(self.seq[sp])
        self.sig_snap[sp].append(dict(self.seen[sp]))
        self.last[sp] = None
        n = self.nsig[sp]
        for e in ENGS:
            if e == sp:
                continue
            self.eng[e].wait_ge(self.sem[sp], n)
            self.seen[e][sp] = n
            for e3, v in self.seen[sp].items():
                if v > self.seen[e][e3]:
                    self.seen[e][e3] = v
            for sid, v in self.seen_d[sp].items():
                if v > self.seen_d[e].get(sid, 0):
                    self.seen_d[e][sid] = v

    def finish(self, out_regs):
        for d in out_regs:
            self._wait("sp", ("d", d.dsem, d.dcnt, id(d)))


class WStream:
    def __init__(self, k, specs, es, nbuf=3, pf=2, q="pool", name="wb"):
        self.k = k
        self.specs = specs
        self.nbuf = nbuf
        self.pf = pf
        self.q = q
        self.bufs = [k.sb("%s%d" % (name, i), [128, 8, 512], BF16, es) for i in range(nbuf)]
        self.regs = [Reg() for _ in range(nbuf)]
        self.issued = 0
        self.cur = 0

    def _issue(self, i):
        buf = self.bufs[i % self.nbuf]
        reg = self.regs[i % self.nbuf]
        for (kc0, c0, src, nk, ncols) in self.specs[i][1]:
            self.k.dma(self.q, buf[:, kc0:kc0 + nk, c0:c0 + ncols],
                       src.rearrange("(kc p) n -> p kc n", p=128), wr=[reg])

    def next(self, tag):
        i = self.cur
        assert self.specs[i][0] == tag, (self.specs[i][0], tag)
        lim = min(len(self.specs), i + 1 + self.pf)
        while self.issued < lim:
            self._issue(self.issued)
            self.issued += 1
        self.cur += 1
        return self.bufs[i % self.nbuf], self.regs[i % self.nbuf]


def host_consts():
    c = {}
    inv = (1.0 / (np.float32(10000.0) ** (np.arange(0, 256, 2, dtype=np.float32) / np.float32(256)))).astype(np.float32)
    pos = np.arange(S, dtype=np.float32)
    ang = (pos[:, None] * inv[None, :]).astype(np.float32)
    c["rcos"] = np.ascontiguousarray(np.cos(ang).astype(np.float32).T)
    c["rsin"] = np.ascontiguousarray(np.sin(ang).astype(np.float32).T)
    inv2 = (1.0 / (np.float32(500000.0) ** (np.arange(0, 16, 2, dtype=np.float32) / np.float32(16)))).astype(np.float32)
    ang2 = (pos[:, None] * inv2[None, :]).astype(np.float32)
    c["dcs"] = np.concatenate([np.tile(np.cos(ang2), (1, 8)), np.tile(np.sin(ang2), (1, 8))], axis=1).astype(np.float32)
    cst = np.zeros((128, 32), np.float32)
    maskT = np.zeros((128, 4, 128), np.float32)
    n = np.arange(128, dtype=np.float64)
    cds = []
    for h in range(4):
        g = 1.0 - 2.0 ** (-5.0 - h)
        qd = g ** (n + 1.0)
        cst[:, h] = qd
        cst[:, 4 + h] = g ** (127.0 - n) / 16.0
        cst[:, 8 + h] = qd * qd
        m = n[:, None]
        nn = n[None, :]
        maskT[:, h, :] = np.where(nn >= m, g ** (-(m + 1.0)) / 16.0, 0.0)
        cds.append(float(g ** 128.0))
    cst[:, 12] = -0.5
    cst[:, 13] = EPS
    c["cst"] = cst
    c["maskT"] = maskT
    c["cds"] = cds
    c["ident"] = np.eye(128, dtype=np.float32)
    return c


class _Stop(Exception):
    pass


def build_program(ntiles=8, phases="ABC", debug=False, limit=10**9):
    def stage(n):
        if n > limit:
            raise _Stop()

    nc = bass.Bass("TRN2", target_bir_lowering=False)
    HC = host_consts()
    cds = HC["cds"]

    declared = []

    def inp(name, shape, ph, dt=F32):
        if not (set(ph) & set(phases)):
            return None
        declared.append(name)
        return nc.dram_tensor(name, list(shape), dt, kind="ExternalInput").ap()

    xin = inp("x", [S, D], "A")
    ln_mix = inp("ln_mix", [2, D], "AB")
    ln_ffn = inp("ln_ffn", [2, D], "AC")
    ret_w_in = inp("ret_w_in", [D, 6144], "A")
    ret_w_o = inp("ret_w_o", [2048, D], "A")
    kv_norm = inp("kv_norm", [1, D], "A")
    w_kv = inp("w_kv", [D, 2048], "A")
    w_q = inp("diff_w_q", [D, D], "B")
    lam = inp("lam", [1, 256], "B")
    subln = inp("diff_subln", [1, 128], "B")
    w_do = inp("diff_w_o", [D, D], "B")
    ffn_gu = inp("ffn_w_gu", [D, 2 * FFN], "A")
    ffn_dn = inp("ffn_w_down", [FFN, D], "A")
    router = inp("moe_router", [D, NEXP], "C")
    moe_gu = inp("moe_w_gu", [NEXP, D, 2 * EXD], "C")
    moe_dn = inp("moe_w_down", [NEXP, EXD, D], "C")
    final_norm = inp("final_norm", [1, D], "C")
    rcos = inp("rcos", [128, S], "A")
    rsin = inp("rsin", [128, S], "A")
    dcs = inp("dcs", [S, 128], "A")
    dcs_own = inp("dcs_own", [2048, 128], "B")
    cst_in = inp("cst", [128, 32], "ABC")
    maskT_in = inp("maskT", [128, 4, 128], "A")
    ident_in = inp("ident", [128, 128], "ABC")
    amask_in = inp("amask", [128, 2, 128], "B")
    nc_in_names = declared

    out = nc.dram_tensor("out", [2048, D], F32, kind="ExternalOutput").ap() if "C" in phases else None
    dbg_outs = []

    def scratch(name, shape, dt, prod, cons):
        if prod in phases:
            kind = "ExternalOutput" if debug else "Internal"
        elif cons in phases:
            kind = "ExternalInput"
            declared.append(name)
        else:
            return None
        return nc.dram_tensor(name, list(shape), dt, kind=kind).ap()

    x1s = scratch("x1s", [S, D], F32, "A", "B")
    kTs = scratch("kTs", [8, 128, S], BF16, "A", "B")
    vs = scratch("vs", [S, D], BF16, "A", "B")
    xbs = scratch("xbs", [2048, D], F32, "B", "C")
    xown = nc.dram_tensor("xown", [2048, D], F32, kind="Internal").ap() if "B" in phases else None

    with ExitStack() as es:
        k = KB(nc, es)
        R_x1s = [Reg() for _ in range(8)]
        R_kTs = [Reg() for _ in range(8)]
        R_vs = [Reg() for _ in range(8)]
        R_xbs = [Reg() for _ in range(4)]
        R_xown = [Reg() for _ in range(4)]
        R_out = Reg()

        cst = k.sb("cst", [128, 32], F32); R_cst = Reg()
        ident = k.sb("ident", [128, 128], BF16); R_ident = Reg()
        k.dma("sp", cst[:], cst_in[:, :], wr=[R_cst])
        k.dma("pool", ident[:], ident_in[:, :], wr=[R_ident])

        PB = [k.ps("pb%d" % i, [128, 512], F32) for i in range(7)]
        R_PB = [Reg() for _ in range(7)]
        PT = k.ps("pt", [128, 1024], BF16); R_PT = Reg()

        def bcast_load(dst, src_row, reg, q="sp"):
            k.dma(q, dst, src_row.partition_broadcast(128), wr=[reg])

        def rmsnorm_to_hT(src_ap, R_src, gain, R_gain, hT_dst, R_hT, tmp, defer=False, sq_on_dve=False):
            (junk, R_junk, ss, R_ss, ve, R_ve, rstd, R_rstd, hn, R_hn) = tmp
            if sq_on_dve:
                k.op("dve", lambda e: e.scalar_tensor_tensor(out=junk[:], in0=src_ap, scalar=1.0, in1=src_ap,
                                                             op0=ALU.mult, op1=ALU.mult, accum_out=ss[:, 0:1]),
                     rd=[R_src], wr=[R_junk, R_ss])
            else:
                k.op("act", lambda e: e.activation(out=junk[:], in_=src_ap, func=AF.Square,
                                                   accum_out=ss[:, 0:1]), rd=[R_src], wr=[R_junk, R_ss])
            k.op("dve", lambda e: e.tensor_scalar(out=ve[:], in0=ss[:], scalar1=1.0 / D, scalar2=EPS,
                                                  op0=ALU.mult, op1=ALU.add), rd=[R_ss], wr=[R_ve])
            k.op("pool", lambda e: e.tensor_tensor(out=rstd[:], in0=ve[:], in1=cst[:, 12:13], op=ALU.pow),
                 rd=[R_ve, R_cst], wr=[R_rstd])
            k.op("dve", lambda e: e.scalar_tensor_tensor(out=hn[:], in0=src_ap, scalar=rstd[:, 0:1], in1=gain,
                                                         op0=ALU.mult, op1=ALU.mult),
                 rd=[R_src, R_rstd, R_gain], wr=[R_hn])
            def back():
                for c in range(8):
                    k.op("pe", lambda e, c=c: e.transpose(out=PT[:, c * 128:(c + 1) * 128],
                                                          in_=hn[:, c * 128:(c + 1) * 128], identity=ident[:]),
                         rd=[R_hn, R_ident], wr=[R_PT])
                k.op("act", lambda e: e.activation(out=hT_dst, in_=PT[:].rearrange("p (c t) -> p c t", c=8),
                                                   func=AF.Copy), rd=[R_PT], wr=[R_hT])
            if defer:
                return back
            back()

        if "A" in phases:
            with ExitStack() as pes:
              try:
                    specs = []
                    for t in range(ntiles):
                        for hd in range(4):
                            specs.append((("qk", t, hd), [(0, 0, ret_w_in[:, hd * 256:(hd + 1) * 256], 8, 256),
                                                          (0, 256, ret_w_in[:, 1024 + hd * 256:1024 + (hd + 1) * 256], 8, 256)]))
                            specs.append((("v", t, hd), [(0, 0, ret_w_in[:, 2048 + hd * 512:2048 + (hd + 1) * 512], 8, 512)]))
                            specs.append((("g", t, hd), [(0, 0, ret_w_in[:, 4096 + hd * 512:4096 + (hd + 1) * 512], 8, 512)]))
                        if t > 0:
                            for nci in range(4):
                                specs.append((("kv", t - 1, nci), [(0, 0, w_kv[:, nci * 512:(nci + 1) * 512], 8, 512)]))
                        for nh in range(2):
                            for kg in range(2):
                                specs.append((("wo", t, nh, kg),
                                              [(0, 0, ret_w_o[kg * 1024:(kg + 1) * 1024, nh * 512:(nh + 1) * 512], 8, 512)]))
                        for j in range(11):
                            specs.append((("gu", t, j), [(0, 0, ffn_gu[:, j * 256:(j + 1) * 256], 8, 256),
                                                         (0, 256, ffn_gu[:, FFN + j * 256:FFN + (j + 1) * 256], 8, 256)]))
                        for nh in range(2):
                            for kg, (k0, nk) in enumerate(((0, 8), (8, 8), (16, 6))):
                                specs.append((("dn", t, nh, kg),
                                              [(0, 0, ffn_dn[k0 * 128:(k0 + nk) * 128, nh * 512:(nh + 1) * 512], nk, 512)]))
                        if t == ntiles - 1:
                            for nci in range(4):
                                specs.append((("kv", t, nci), [(0, 0, w_kv[:, nci * 512:(nci + 1) * 512], 8, 512)]))
                    ws = WStream(k, specs, pes, nbuf=4, pf=3)

                    xtb = [k.sb("xt%d" % i, [128, 4, D], F32, pes) for i in range(2)]
                    R_xtb = [[Reg() for _ in range(4)] for _ in range(2)]
                    hTb = [k.sb("hT%d" % i, [128, 8, 512], BF16, pes) for i in range(2)]
                    R_hTb = [[Reg() for _ in range(4)] for _ in range(2)]
                    xt, R_xt, hT, R_hT = xtb[0], R_xtb[0], hTb[0], R_hTb[0]
                    qk4 = k.sb("qk4", [128, 4, 2, 512], BF16, pes)
                    qT2 = [qk4[:, i, :, :] for i in range(2)]; R_qT2 = [Reg(), Reg()]
                    kT2 = [qk4[:, 2 + i, :, :] for i in range(2)]; R_kT2 = [Reg(), Reg()]
                    ktm2 = [k.sb("ktm%d" % i, [128, 4, 256], BF16, pes) for i in range(2)]; R_ktm2 = [Reg(), Reg()]
                    vtm2 = [k.sb("vtm%d" % i, [128, 4, 512], BF16, pes) for i in range(2)]
                    R_vtm2 = [[Reg() for _ in range(4)] for _ in range(2)]
                    sg2 = [k.sb("sg%d" % i, [128, 4, 512], BF16, pes) for i in range(2)]
                    R_sg2 = [[Reg() for _ in range(4)] for _ in range(2)]
                    st = k.sb("st", [128, 8, 512], F32, pes); R_st = [Reg() for _ in range(8)]
                    stb = k.sb("stb", [128, 8, 512], BF16, pes); R_stb = [Reg() for _ in range(8)]
                    actT = k.sb("actT", [128, 22, 512], BF16, pes); R_actT = Reg()
                    ogT = actT[:, 0:16, :]; R_ogT = R_actT
                    gains = k.sb("gains", [128, 3, D], F32, pes); R_gains = Reg()
                    maskT = k.sb("maskT", [128, 4, 128], F32, pes); R_maskT = Reg()
                    cs = k.sb("cs", [128, 2, 512], F32, pes); R_cs = Reg()
                    dcsT = k.sb("dcsT", [128, 4, 128], F32, pes); R_dcsT = Reg()
                    rt = [k.sb("rt%d" % i, [128, 512], F32, pes) for i in range(4)]; R_rt = [Reg() for _ in range(4)]
                    scT = k.sb("scT", [128, 128], BF16, pes); R_scT = Reg()
                    on = k.sb("on", [128, 512], F32, pes); R_on = Reg()
                    og2 = [k.sb("og%d" % i, [128, 512], BF16, pes) for i in range(2)]; R_og2 = [Reg(), Reg()]
                    sgf2 = [k.sb("sgf%d" % i, [128, 512], BF16, pes) for i in range(2)]; R_sgf2 = [Reg(), Reg()]
                    kk2 = [k.sb("kk%d" % i, [128, 512], BF16, pes) for i in range(2)]; R_kk2 = [Reg(), Reg()]
                    kr2 = [[k.sb("kr%d_%d" % (j, i), [128, 8, 8], F32, pes) for i in range(4)] for j in range(2)]
                    R_kr2 = [[Reg() for _ in range(4)] for _ in range(2)]
                    kst2 = [k.sb("kst%d" % i, [128, 4, 128], BF16, pes) for i in range(2)]; R_kst2 = [Reg(), Reg()]
                    vst2 = [k.sb("vst%d" % i, [128, 512], BF16, pes) for i in range(2)]; R_vst2 = [Reg(), Reg()]
                    sm = k.sb("sm", [128, 16], F32, pes); R_sm = [Reg() for _ in range(16)]
                    tmps = []
                    junk_sh = k.sb("junk", [128, D], BF16, pes)
                    for i in range(2):
                        junk_i = junk_sh
                        hn_i = k.sb("hn%d" % i, [128, D], BF16, pes)
                        smn = k.sb("smn%d" % i, [128, 4], F32, pes)
                        tmps.append((junk_i, Reg(), smn[:, 0:1], Reg(), smn[:, 1:2], Reg(), smn[:, 2:3], Reg(), hn_i, Reg()))

                    print('SBUF remaining phase A', nc.sbuf_bytes_remaining)
                    bcast_load(gains[:, 0, :], ln_mix[0:1, :], R_gains)
                    bcast_load(gains[:, 1, :], ln_ffn[0:1, :], R_gains)
                    bcast_load(gains[:, 2, :], kv_norm[0:1, :], R_gains)
                    k.dma("sp", maskT[:], maskT_in[:, :, :], wr=[R_maskT])
                    k.op("dve", lambda e: e.memset(st[:], 0.0), wr=R_st)
                    k.op("pool", lambda e: e.memset(stb[:], 0.0), wr=R_stb)
                    stage(1)

                    def gemm_T_acc(tagbase, lhs_of, R_lhs, kgroups, post):
                        for nh in range(2):
                            for kg, nk in enumerate(kgroups):
                                wb, R_w = ws.next(tagbase + (nh, kg))
                                k0 = sum(kgroups[:kg])
                                for b in range(4):
                                    for kc in range(nk):
                                        first = (kg == 0 and kc == 0)
                                        lastm = (kg == len(kgroups) - 1 and kc == nk - 1)
                                        k.op("pe", lambda e, b=b, kc=kc, first=first, lastm=lastm, wb=wb, k0=k0:
                                             e.matmul(PB[b][:], lhsT=lhs_of(k0 + kc, b), rhs=wb[:, kc, :],
                                                      start=first, stop=lastm),
                                             rd=[R_w] + R_lhs, wr=[R_PB[b]])
                            for b in range(4):
                                post(nh, b, PB[b], R_PB[b])

                    def resid_add(nh, b, P, R_P):
                        k.op("dve", lambda e: e.tensor_tensor(out=xt[:, b, nh * 512:(nh + 1) * 512],
                                                              in0=xt[:, b, nh * 512:(nh + 1) * 512], in1=P[:], op=ALU.add),
                             rd=[R_P, R_xt[b]], wr=[R_xt[b]])

                    def load_x(tt):
                        k.dma("sp", xtb[tt % 2][:], xin[tt * 512:(tt + 1) * 512, :].rearrange("(b p) d -> p b d", p=128),
                              wr=R_xtb[tt % 2])

                    def load_cs(tt):
                        k.dma("sp", cs[:, 0, :], rcos[:, tt * 512:(tt + 1) * 512], wr=[R_cs])
                        k.dma("sp", cs[:, 1, :], rsin[:, tt * 512:(tt + 1) * 512], wr=[R_cs])

                    def load_dcs(tt):
                        k.dma("sp", dcsT[:], dcs[tt * 512:(tt + 1) * 512, :].rearrange("(b p) d -> p b d", p=128), wr=[R_dcsT])

                    def norm0(tt):
                        for b in range(4):
                            rmsnorm_to_hT(xtb[tt % 2][:, b, :], R_xtb[tt % 2][b], gains[:, 0, :], R_gains,
                                          hTb[tt % 2][:, :, b * 128:(b + 1) * 128], R_hTb[tt % 2][b], tmps[b % 2])

                    kv_pending = []
                    load_x(0)
                    load_cs(0)
                    load_dcs(0)
                    norm0(0)
                    for t in range(ntiles):
                        ts0 = t * 512
                        xt, R_xt, hT, R_hT = xtb[t % 2], R_xtb[t % 2], hTb[t % 2], R_hTb[t % 2]
                        if t + 1 < ntiles:
                            load_x(t + 1)
                        stage(2)
                        stage(3)
                        def make_pieces(hd):
                            pi = hd % 2
                            qTc, kTc, ktmc, vtmc, sgc_ = qT2[pi], kT2[pi], ktm2[pi], vtm2[pi], sg2[pi]
                            R_qTc, R_kTc, R_ktmc, R_vtmc, R_sgc_ = R_qT2[pi], R_kT2[pi], R_ktm2[pi], R_vtm2[pi], R_sg2[pi]
                            stt = {}

                            def proj_mm(which, dc, bank):
                                wb, R_w = stt["w"]
                                P, R_P = PB[bank], R_PB[bank]
                                for kc in range(8):
                                    c0 = which * 256 + dc * 128
                                    k.op("pe", lambda e, kc=kc, c0=c0: e.matmul(P[:], lhsT=wb[:, kc, c0:c0 + 128], rhs=hT[:, kc, :],
                                                                                start=(kc == 0), stop=(kc == 7)),
                                         rd=[R_w] + R_hT, wr=[R_P])

                            def rope_ops(dst, R_dst, ba, bb):
                                Pa, Pb, R_Pa, R_Pb = PB[ba], PB[bb], R_PB[ba], R_PB[bb]
                                k.op("dve", lambda e: e.tensor_tensor(out=rt[0][:], in0=Pa[:], in1=cs[:, 0, :], op=ALU.mult),
                                     rd=[R_Pa, R_cs], wr=[R_rt[0]])
                                k.op("dve", lambda e: e.tensor_tensor(out=rt[1][:], in0=Pb[:], in1=cs[:, 1, :], op=ALU.mult),
                                     rd=[R_Pb, R_cs], wr=[R_rt[1]])
                                k.op("dve", lambda e: e.tensor_tensor(out=rt[2][:], in0=Pb[:], in1=cs[:, 0, :], op=ALU.mult),
                                     rd=[R_Pb, R_cs], wr=[R_rt[2]])
                                k.op("dve", lambda e: e.tensor_tensor(out=rt[3][:], in0=Pa[:], in1=cs[:, 1, :], op=ALU.mult),
                                     rd=[R_Pa, R_cs], wr=[R_rt[3]])
                                k.op("pool", lambda e: e.tensor_tensor(out=dst[:, 0, :], in0=rt[0][:], in1=rt[1][:], op=ALU.subtract),
                                     rd=[R_rt[0], R_rt[1]], wr=[R_dst])
                                k.op("pool", lambda e: e.tensor_tensor(out=dst[:, 1, :], in0=rt[2][:], in1=rt[3][:], op=ALU.add),
                                     rd=[R_rt[2], R_rt[3]], wr=[R_dst])

                            def h_q0():
                                stt["w"] = ws.next(("qk", t, hd))
                                proj_mm(0, 0, 0)

                            def h_q1():
                                proj_mm(0, 1, 1)
                                rope_ops(qTc, R_qTc, 0, 1)

                            def h_k0():
                                proj_mm(1, 0, 2)

                            def h_k1():
                                proj_mm(1, 1, 0)
                                rope_ops(kTc, R_kTc, 2, 0)

                            def ktm_transposes():
                                for b in range(4):
                                    for dc in range(2):
                                        k.op("pe", lambda e, b=b, dc=dc: e.transpose(
                                            out=PT[:, b * 256 + dc * 128: b * 256 + (dc + 1) * 128],
                                            in_=kTc[:, dc, b * 128:(b + 1) * 128], identity=ident[:]),
                                            rd=[R_kTc, R_ident], wr=[R_PT])
                                k.op("act", lambda e: e.activation(out=ktmc[:].rearrange("p b d -> p (b d)"), in_=PT[:],
                                                                   func=AF.Copy, scale=cst[:, 4 + hd:5 + hd]),
                                     rd=[R_PT, R_cst], wr=[R_ktmc])

                            def half_vg(tag, dstt, R_d, fn, half):
                                def f():
                                    if half == 0:
                                        stt[tag] = ws.next((tag, t, hd))
                                    if tag == "g" and half == 0:
                                        ktm_transposes()
                                    wb, R_w = stt[tag]
                                    for b in (0, 1) if half == 0 else (2, 3):
                                        bi = (b + (2 if tag == "g" else 0)) % 3
                                        P, R_P = PB[bi], R_PB[bi]
                                        for kc in range(8):
                                            k.op("pe", lambda e, b=b, kc=kc, P=P: e.matmul(P[:], lhsT=hT[:, kc, b * 128:(b + 1) * 128],
                                                                                           rhs=wb[:, kc, :], start=(kc == 0), stop=(kc == 7)),
                                                 rd=[R_w, R_hT[b]], wr=[R_P])
                                        k.op("act", lambda e, b=b, P=P: e.activation(out=dstt[:, b, :], in_=P[:], func=fn),
                                             rd=[R_P], wr=[R_d[b]])
                                return f
                            return [h_q0, h_q1, h_k0, h_k1,
                                    half_vg("v", vtmc, R_vtmc, AF.Copy, 0), half_vg("v", vtmc, R_vtmc, AF.Copy, 1),
                                    half_vg("g", sgc_, R_sgc_, AF.Silu, 0), half_vg("g", sgc_, R_sgc_, AF.Silu, 1)]

                        def rec_sd(hd, b):
                            pi = hd % 2
                            qTc, kTc, ktmc, vtmc = qT2[pi], kT2[pi], ktm2[pi], vtm2[pi]
                            R_qTc, R_kTc, R_ktmc, R_vtmc = R_qT2[pi], R_kT2[pi], R_ktm2[pi], R_vtm2[pi]
                            bs = slice(b * 128, (b + 1) * 128)
                            Psc, R_Psc = PB[3], R_PB[3]
                            Po, R_Po = PB[4], R_PB[4]
                            for dc in range(2):
                                k.op("pe", lambda e, dc=dc: e.matmul(Psc[:, 0:128], lhsT=kTc[:, dc, bs], rhs=qTc[:, dc, bs],
                                                                     start=(dc == 0), stop=(dc == 1)),
                                     rd=[R_kTc, R_qTc], wr=[R_Psc])
                            k.op("dve", lambda e: e.tensor_tensor(out=scT[:], in0=Psc[:, 0:128], in1=maskT[:, hd, :], op=ALU.mult),
                                 rd=[R_Psc, R_maskT], wr=[R_scT])
                            for dc in range(2):
                                Pd, R_Pd = PB[5 + dc], R_PB[5 + dc]
                                k.op("pe", lambda e, dc=dc, Pd=Pd: e.matmul(Pd[:], lhsT=ktmc[:, b, dc * 128:(dc + 1) * 128],
                                                                            rhs=vtmc[:, b, :], start=True, stop=True),
                                     rd=[R_ktmc, R_vtmc[b]], wr=[R_Pd])

                        def rec_o(hd, b):
                            pi = hd % 2
                            qTc, kTc, ktmc, vtmc = qT2[pi], kT2[pi], ktm2[pi], vtm2[pi]
                            R_qTc, R_kTc, R_ktmc, R_vtmc = R_qT2[pi], R_kT2[pi], R_ktm2[pi], R_vtm2[pi]
                            bs = slice(b * 128, (b + 1) * 128)
                            Po, R_Po = PB[4], R_PB[4]
                            k.op("pe", lambda e: e.matmul(Po[:], lhsT=scT[:], rhs=vtmc[:, b, :], start=True, stop=False),
                                 rd=[R_scT, R_vtmc[b]], wr=[R_Po])
                            for dc in range(2):
                                k.op("pe", lambda e, dc=dc: e.matmul(Po[:], lhsT=qTc[:, dc, bs], rhs=stb[:, hd * 2 + dc, :],
                                                                     start=False, stop=(dc == 1)),
                                     rd=[R_qTc, R_stb[hd * 2 + dc]], wr=[R_Po])
                            k.op("dve", lambda e: e.bn_stats(out=sm[:, 3:9], in_=Po[:]), rd=[R_Po], wr=[R_sm[3]])
                            k.op("dve", lambda e: e.bn_aggr(out=sm[:, 9:11], in_=sm[:, 3:9]), rd=[R_sm[3]], wr=[R_sm[4]])
                            k.op("dve", lambda e: e.tensor_scalar(out=sm[:, 11:12], in0=sm[:, 10:11],
                                                                  scalar1=cst[:, 8 + hd:9 + hd], scalar2=EPS,
                                                                  op0=ALU.mult, op1=ALU.add),
                                 rd=[R_sm[4], R_cst], wr=[R_sm[5]])
                            k.op("pool", lambda e: e.tensor_tensor(out=sm[:, 12:13], in0=sm[:, 11:12], in1=cst[:, 12:13],
                                                                   op=ALU.pow), rd=[R_sm[5], R_cst], wr=[R_sm[6]])
                            k.op("dve", lambda e: e.tensor_tensor(out=sm[:, 13:14], in0=sm[:, 12:13],
                                                                  in1=cst[:, hd:hd + 1], op=ALU.mult),
                                 rd=[R_sm[6], R_cst], wr=[R_sm[7]])
                            k.op("dve", lambda e: e.scalar_tensor_tensor(out=sm[:, 14:15], in0=sm[:, 9:10], scalar=-1.0,
                                                                         in1=sm[:, 13:14], op0=ALU.mult, op1=ALU.mult),
                                 rd=[R_sm[4], R_sm[7]], wr=[R_sm[8]])
                            k.op("act", lambda e: e.activation(out=on[:], in_=Po[:], func=AF.Identity,
                                                               bias=sm[:, 14:15], scale=sm[:, 13:14]),
                                 rd=[R_Po, R_sm[7], R_sm[8]], wr=[R_on])
                            k.op("pool", lambda e: e.tensor_tensor(out=og2[b % 2][:], in0=on[:], in1=sg2[pi][:, b, :], op=ALU.mult),
                                 rd=[R_on, R_sg2[pi][b]], wr=[R_og2[b % 2]])
                            for dc in range(2):
                                Pd, R_Pd = PB[5 + dc], R_PB[5 + dc]
                                si = hd * 2 + dc
                                k.op("dve", lambda e, si=si, Pd=Pd: e.scalar_tensor_tensor(
                                    out=st[:, si, :], in0=st[:, si, :], scalar=cds[hd], in1=Pd[:],
                                    op0=ALU.mult, op1=ALU.add), rd=[R_Pd, R_st[si]], wr=[R_st[si]])
                                k.op("act", lambda e, si=si: e.activation(out=stb[:, si, :], in_=st[:, si, :], func=AF.Copy),
                                     rd=[R_st[si]], wr=[R_stb[si]])

                        def rec_back(hd, b):
                            bs = slice(b * 128, (b + 1) * 128)
                            for c in range(4):
                                k.op("pe", lambda e, c=c: e.transpose(out=PT[:, c * 128:(c + 1) * 128],
                                                                      in_=og2[b % 2][:, c * 128:(c + 1) * 128], identity=ident[:]),
                                     rd=[R_og2[b % 2], R_ident], wr=[R_PT])
                            k.op("act", lambda e: e.activation(
                                out=ogT[:, hd * 4:(hd + 1) * 4, bs],
                                in_=PT[:, 0:512].rearrange("p (c t) -> p c t", c=4), func=AF.Copy),
                                rd=[R_PT], wr=[R_ogT])

                        for pc in make_pieces(0):
                            pc()
                        pend_back = None
                        for hd in range(4):
                            nxt = make_pieces(hd + 1) if hd < 3 else [None] * 8
                            for b in range(4):
                                rec_sd(hd, b)
                                if nxt[2 * b] is not None:
                                    nxt[2 * b]()
                                elif kv_pending:
                                    kv_pending.pop(0)()
                                    kv_pending.pop(0)()
                                rec_o(hd, b)
                                if nxt[2 * b + 1] is not None:
                                    nxt[2 * b + 1]()
                                elif kv_pending:
                                    kv_pending.pop(0)()
                                    kv_pending.pop(0)()
                                    if not kv_pending:
                                        load_dcs(t)
                                if pend_back is not None:
                                    rec_back(*pend_back)
                                pend_back = (hd, b)
                        rec_back(*pend_back)
                        stage(7)
                        if t + 1 < ntiles:
                            load_cs(t + 1)
                        gemm_T_acc(("wo", t), lambda kc, b: ogT[:, kc, b * 128:(b + 1) * 128], [R_ogT], [8, 8], resid_add)
                        stage(8)
                        for b in range(4):
                            rmsnorm_to_hT(xt[:, b, :], R_xt[b], gains[:, 1, :], R_gains,
                                          hT[:, :, b * 128:(b + 1) * 128], R_hT[b], tmps[b % 2], sq_on_dve=(b % 2 == 1))
                        for j in range(11):
                            wb, R_w = ws.next(("gu", t, j))
                            for fi in range(2):
                                gp = fi
                                Pg, R_Pg = (PB[4], R_PB[4]) if gp == 0 else (PB[6], R_PB[6])
                                Pu, R_Pu = (PB[5], R_PB[5]) if gp == 0 else (PB[3], R_PB[3])
                                sgf, R_sgf = sgf2[gp], R_sgf2[gp]
                                for P, R_P, c0 in ((Pg, R_Pg, fi * 128), (Pu, R_Pu, 256 + fi * 128)):
                                    for kc in range(8):
                                        k.op("pe", lambda e, kc=kc, P=P, c0=c0, wb=wb:
                                             e.matmul(P[:], lhsT=wb[:, kc, c0:c0 + 128], rhs=hT[:, kc, :],
                                                      start=(kc == 0), stop=(kc == 7)),
                                             rd=[R_w] + R_hT, wr=[R_P])
                                k.op("act", lambda e: e.activation(out=sgf[:], in_=Pg[:], func=AF.Silu),
                                     rd=[R_Pg], wr=[R_sgf])
                                fc = j * 2 + fi
                                k.op("dve", lambda e, fc=fc: e.tensor_tensor(out=actT[:, fc, :], in0=Pu[:], in1=sgf[:],
                                                                            op=ALU.mult),
                                     rd=[R_Pu, R_sgf], wr=[R_actT])
                        if t + 1 < ntiles:
                            norm0(t + 1)
                        gemm_T_acc(("dn", t), lambda kc, b: actT[:, kc, b * 128:(b + 1) * 128], [R_actT], [8, 8, 6], resid_add)
                        stage(9)
                        k.dma("sp", x1s[ts0:ts0 + 512, :].rearrange("(b p) d -> p b d", p=128), xt[:],
                              rd=R_xt, wr=[R_x1s[t]])
                        stage(10)
                        for b in range(4):
                            rmsnorm_to_hT(xt[:, b, :], R_xt[b], gains[:, 2, :], R_gains,
                                          hT[:, :, b * 128:(b + 1) * 128], R_hT[b], tmps[b % 2], sq_on_dve=(b % 2 == 1))

                        def make_kv_units(t=t, hTt=hT, R_hTt=R_hT):
                            stt = {"pend": None, "cnt": 0}
                            ts0k = t * 512

                            def unit(nci, b):
                                def f():
                                    if b == 0:
                                        stt["w"] = ws.next(("kv", t, nci))
                                    wb, R_w = stt["w"]
                                    u = stt["cnt"]
                                    stt["cnt"] += 1
                                    P, R_P = PB[u % 3], R_PB[u % 3]
                                    for kc in range(8):
                                        k.op("pe", lambda e, kc=kc: e.matmul(P[:], lhsT=hTt[:, kc, b * 128:(b + 1) * 128], rhs=wb[:, kc, :],
                                                                             start=(kc == 0), stop=(kc == 7)),
                                             rd=[R_w, R_hTt[b]], wr=[R_P])
                                    if nci < 2:
                                        pr = u % 2
                                        kkc, R_kkc = kk2[pr], R_kk2[pr]
                                        ra, R_ra = rt[2 * pr], R_rt[2 * pr]
                                        rb, R_rb = rt[2 * pr + 1], R_rt[2 * pr + 1]
                                        krc, R_krc = kr2[pr], R_kr2[pr]
                                        kst, R_kst = kst2[pr], R_kst2[pr]
                                        k.op("act", lambda e: e.activation(out=ra[:], in_=P[:], func=AF.Copy), rd=[R_P], wr=[R_ra])
                                        P3 = ra[:].rearrange("p (g d) -> p g d", d=64)
                                        kk3 = kkc[:].rearrange("p (g d) -> p g d", d=64)
                                        k.op("act", lambda e: e.activation(out=rb[:, 0:128], in_=dcsT[:, b, :], func=AF.Copy),
                                             rd=[R_dcsT], wr=[R_rb])
                                        cosb = rb[:, 0:64].rearrange("p (g d) -> p g d", d=8)
                                        sinb = rb[:, 64:128].rearrange("p (g d) -> p g d", d=8)
                                        k.op("act", lambda e: e.activation(out=kkc[:], in_=P[:], func=AF.Copy), rd=[R_P], wr=[R_kkc])
                                        for i, (lo, tb) in enumerate(((0, cosb), (8, sinb), (8, cosb), (0, sinb))):
                                            k.op("dve", lambda e, i=i, lo=lo, tb=tb: e.tensor_tensor(out=krc[i][:], in0=P3[:, :, lo:lo + 8], in1=tb,
                                                                                                      op=ALU.mult),
                                                 rd=[R_ra, R_rb], wr=[R_krc[i]])
                                        k.op("dve", lambda e: e.tensor_tensor(out=kk3[:, :, 0:8], in0=krc[0][:], in1=krc[1][:], op=ALU.subtract),
                                             rd=[R_krc[0], R_krc[1]], wr=[R_kkc])
                                        k.op("dve", lambda e: e.tensor_tensor(out=kk3[:, :, 8:16], in0=krc[2][:], in1=krc[3][:], op=ALU.add),
                                             rd=[R_krc[2], R_krc[3]], wr=[R_kkc])

                                        def back():
                                            for c in range(4):
                                                k.op("pe", lambda e, c=c: e.transpose(out=PT[:, c * 128:(c + 1) * 128],
                                                                                      in_=kkc[:, c * 128:(c + 1) * 128], identity=ident[:]),
                                                     rd=[R_kkc, R_ident], wr=[R_PT])
                                            k.op("act", lambda e: e.activation(out=kst[:], in_=PT[:, 0:512].rearrange("p (c t) -> p c t", c=4),
                                                                               func=AF.Copy), rd=[R_PT], wr=[R_kst])
                                            k.dma("sp", kTs[nci * 4:(nci + 1) * 4, :, ts0k + b * 128:ts0k + (b + 1) * 128].rearrange("h p s -> p h s"),
                                                  kst[:], rd=[R_kst], wr=[R_kTs[t]])
                                        if stt["pend"] is not None:
                                            stt["pend"]()
                                        stt["pend"] = back
                                    else:
                                        if stt["pend"] is not None:
                                            stt["pend"]()
                                            stt["pend"] = None
                                        pr = u % 2
                                        vstc, R_vstc = vst2[pr], R_vst2[pr]
                                        c0 = (nci - 2) * 512
                                        k.op("act", lambda e: e.activation(out=vstc[:], in_=P[:], func=AF.Copy), rd=[R_P], wr=[R_vstc])
                                        k.dma("sp", vs[ts0k + b * 128:ts0k + (b + 1) * 128, c0:c0 + 512], vstc[:], rd=[R_vstc], wr=[R_vs[t]])
                                return f
                            return [unit(nci, b) for nci in range(4) for b in range(4)]

                        kv_pending = make_kv_units()
                        if t == ntiles - 1:
                            for u_ in kv_pending:
                                u_()
                            kv_pending = []
              except _Stop:
                pass
              k.barrier()

        if "B" in phases:
            with ExitStack() as pes:
              try:
                hT2 = k.sb("hT2", [128, 8, 2048], BF16, pes); R_hT2 = [Reg() for _ in range(4)]
                ao = hT2[:].rearrange("p c t -> p (c t)").rearrange("p (j d) -> p j d", d=D)
                R_ao = Reg()
                bjunk = k.sb("bjunk", [128, D], BF16, pes); R_bjunk = Reg()
                bhn = k.sb("bhn", [128, D], BF16, pes); R_bhn = Reg()
                bsm = k.sb("bsm", [128, 24], F32, pes); R_bsm = [Reg() for _ in range(24)]
                bhn1 = k.sb("bhn1", [128, D], BF16, pes); R_bhn1 = Reg()
                bsmb = k.sb("bsmb", [128, 4], F32, pes)
                btmps = [(bjunk, R_bjunk, bsm[:, 0:1], R_bsm[0], bsm[:, 1:2], R_bsm[1], bsm[:, 2:3], R_bsm[2], bhn, R_bhn),
                         (bjunk, Reg(), bsmb[:, 0:1], Reg(), bsmb[:, 1:2], Reg(), bsmb[:, 2:3], Reg(), bhn1, R_bhn1)]
                xst = [k.sb("xst%d" % i, [128, 2, D], F32, pes) for i in range(2)]; R_xst = [Reg(), Reg()]
                xsel2 = [k.sb("xsel%d" % i, [128, D], F32, pes) for i in range(2)]; R_xsel2 = [Reg(), Reg()]
                gain1 = k.sb("gain1", [128, D], F32, pes); R_gain1 = Reg()
                lamt = k.sb("lamt", [128, 256], F32, pes); R_lamt = Reg()
                gsub = k.sb("gsub", [128, 128], F32, pes); R_gsub = Reg()
                amask = k.sb("amask", [128, 256], BF16, pes); R_amask = Reg()
                bcast_load(gain1[:], ln_mix[1:2, :], R_gain1)
                bcast_load(lamt[:], lam[0:1, :], R_lamt)
                bcast_load(gsub[:], subln[0:1, :], R_gsub)
                k.dma("pool", amask[:], amask_in[:, :, :].rearrange("p a q -> p (a q)"), wr=[R_amask])
                k.op("dve", lambda e: e.tensor_scalar(out=gsub[:], in0=gsub[:], scalar1=1.0 - LAMBDA_INIT, scalar2=None,
                                                      op0=ALU.mult), rd=[R_gsub], wr=[R_gsub])
                for i in range(2):
                    k.op("dve", lambda e, i=i: e.tensor_tensor(out=lamt[:, i * 128:i * 128 + 64], in0=lamt[:, i * 128:i * 128 + 64],
                                                               in1=lamt[:, i * 128 + 64:i * 128 + 128], op=ALU.mult),
                         rd=[R_lamt], wr=[R_lamt])
                    k.op("dve", lambda e, i=i: e.reduce_sum(out=bsm[:, 3 + i:4 + i], in_=lamt[:, i * 128:i * 128 + 64],
                                                            axis=mybir.AxisListType.X), rd=[R_lamt], wr=[R_bsm[3 + i]])
                    k.op("act", lambda e, i=i: e.activation(out=bsm[:, 5 + i:6 + i], in_=bsm[:, 3 + i:4 + i], func=AF.Exp),
                         rd=[R_bsm[3 + i]], wr=[R_bsm[5 + i]])
                k.op("dve", lambda e: e.scalar_tensor_tensor(out=bsm[:, 7:8], in0=bsm[:, 6:7], scalar=-LAMBDA_INIT, in1=bsm[:, 5:6],
                                                             op0=ALU.add, op1=ALU.subtract),
                     rd=[R_bsm[5], R_bsm[6]], wr=[R_bsm[7]])
                NEGLAM = bsm[:, 7:8]; R_NEGLAM = R_bsm[7]

                def load_own_block(j, dst_ap, R_dst):
                    xs, R_xs = xst[j % 2], R_xst[j % 2]
                    k.dma("sp", xs[:], x1s[2 * j * 128:(2 * j + 2) * 128, :].rearrange("(r p) d -> p r d", p=128),
                          rd=[R_x1s[(2 * j) // 4]], wr=[R_xs])
                    k.op("dve", lambda e: e.tensor_scalar(out=dst_ap, in0=xs[:, 0, :], scalar1=cst[:, 14:15], scalar2=None,
                                                          op0=ALU.mult), rd=[R_xs, R_cst], wr=[R_dst])
                    k.op("dve", lambda e: e.scalar_tensor_tensor(out=dst_ap, in0=xs[:, 1, :], scalar=cst[:, 15:16], in1=dst_ap,
                                                                 op0=ALU.mult, op1=ALU.add),
                         rd=[R_xs, R_cst, R_dst], wr=[R_dst])
                    k.dma("sp", xown[j * 128:(j + 1) * 128, :], dst_ap, rd=[R_dst], wr=[R_xown[j // 4]])

                with ExitStack() as pes2:
                    specs = []
                    for g in range(4):
                        for nh in range(2):
                            specs.append((("wq", g, nh), [(0, 0, w_q[:, nh * 512:(nh + 1) * 512], 8, 512)]))
                    ws = WStream(k, specs, pes2, name="wbB")
                    qTa = k.sb("qTa", [128, 8, 2048], BF16, pes2); R_qTa = [Reg() for _ in range(16)]
                    dq = k.sb("dq", [128, 4, 128], F32, pes2); R_dq = Reg()
                    brt = [k.sb("brt%d" % i, [128, 512], F32, pes2) for i in range(4)]; R_brt = [Reg() for _ in range(4)]
                    qq2 = [k.sb("qq%d" % i, [128, 512], BF16, pes2) for i in range(2)]; R_qq2 = [Reg(), Reg()]
                    bkr2 = [[k.sb("bkr%d_%d" % (jj, i), [128, 8, 8], F32, pes2) for i in range(4)] for jj in range(2)]
                    R_bkr2 = [[Reg() for _ in range(4)] for _ in range(2)]
                    KT = [k.sb("KT%d" % i, [128, S], BF16, pes2) for i in range(2)]; R_KT = [Reg(), Reg()]
                    VX = [k.sb("VX%d" % i, [128, 32, 132], BF16, pes2) for i in range(2)]; R_VX = [Reg(), Reg()]
                    PTs = [[k.sb("PTs%d%d" % (c, i), [128, 32 * 128], BF16, pes2) for i in range(2)] for c in range(2)]
                    R_PTs = [[Reg(), Reg()], [Reg(), Reg()]]
                    tcomb2 = [k.sb("tcomb%d" % i, [128, 128], F32, pes2) for i in range(2)]; R_tcomb2 = [Reg(), Reg()]
                    osb2 = [k.sb("osb%d" % i, [128, 128], F32, pes2) for i in range(2)]; R_osb2 = [Reg(), Reg()]
                    bsm2 = [k.sb("bsm2_%d" % i, [128, 8], F32, pes2) for i in range(2)]
                    R_bsm2 = [[Reg() for _ in range(8)] for _ in range(2)]
                    ojunk = k.sb("ojunk", [128, 128], BF16, pes2); R_ojunk = Reg()
                    for i in range(2):
                        k.op("pool", lambda e, i=i: e.memset(VX[i][:, :, 128:129], 1.0), wr=[R_VX[i]])
                    pend = None
                    qc = 0

                    def prep_front(j):
                        load_own_block(j, xsel2[j % 2][:], R_xsel2[j % 2])
                        return rmsnorm_to_hT(xsel2[j % 2][:], R_xsel2[j % 2], gain1[:], R_gain1, hT2[:, :, j * 128:(j + 1) * 128],
                                             R_hT2[j // 4], btmps[j % 2], defer=True)
                    for b in range(4):
                        prep_front(b)()
                    for g in range(4):
                        k.dma("sp", dq[:], dcs_own[g * 512:(g + 1) * 512, :].rearrange("(b p) d -> p b d", p=128), wr=[R_dq])
                        backs = {}
                        kitem = 0
                        for nh in range(2):
                            wb, R_w = ws.next(("wq", g, nh))
                            for b in range(4):
                                j = 4 * g + b
                                P, R_P = PB[b], R_PB[b]
                                for kc in range(8):
                                    k.op("pe", lambda e, kc=kc, P=P, wb=wb, j=j:
                                         e.matmul(P[:], lhsT=hT2[:, kc, j * 128:(j + 1) * 128], rhs=wb[:, kc, :],
                                                  start=(kc == 0), stop=(kc == 7)),
                                         rd=[R_w, R_hT2[g]], wr=[R_P])
                                pr = qc % 2
                                qc += 1
                                ra, R_ra = brt[2 * pr], R_brt[2 * pr]
                                rb, R_rb = brt[2 * pr + 1], R_brt[2 * pr + 1]
                                qqc, R_qqc = qq2[pr], R_qq2[pr]
                                krc, R_krc = bkr2[pr], R_bkr2[pr]
                                k.op("act", lambda e, P=P: e.activation(out=ra[:], in_=P[:], func=AF.Copy, scale=0.125),
                                     rd=[R_P], wr=[R_ra])
                                k.op("act", lambda e, b=b: e.activation(out=rb[:, 0:128], in_=dq[:, b, :], func=AF.Copy),
                                     rd=[R_dq], wr=[R_rb])
                                k.op("act", lambda e, P=P: e.activation(out=qqc[:], in_=P[:], func=AF.Copy, scale=0.125),
                                     rd=[R_P], wr=[R_qqc])
                                P3 = ra[:].rearrange("p (g d) -> p g d", d=64)
                                qq3 = qqc[:].rearrange("p (g d) -> p g d", d=64)
                                cosb = rb[:, 0:64].rearrange("p (g d) -> p g d", d=8)
                                sinb = rb[:, 64:128].rearrange("p (g d) -> p g d", d=8)
                                for i, (lo, tb) in enumerate(((0, cosb), (8, sinb), (8, cosb), (0, sinb))):
                                    k.op("dve", lambda e, i=i, lo=lo, tb=tb: e.tensor_tensor(out=krc[i][:], in0=P3[:, :, lo:lo + 8], in1=tb,
                                                                                              op=ALU.mult),
                                         rd=[R_ra, R_rb], wr=[R_krc[i]])
                                k.op("dve", lambda e: e.tensor_tensor(out=qq3[:, :, 0:8], in0=krc[0][:], in1=krc[1][:], op=ALU.subtract),
                                     rd=[R_krc[0], R_krc[1]], wr=[R_qqc])
                                k.op("dve", lambda e: e.tensor_tensor(out=qq3[:, :, 8:16], in0=krc[2][:], in1=krc[3][:], op=ALU.add),
                                     rd=[R_krc[2], R_krc[3]], wr=[R_qqc])

                                def backq(nh=nh, j=j, qqc=qqc, R_qqc=R_qqc):
                                    for c in range(4):
                                        k.op("pe", lambda e, c=c: e.transpose(out=PT[:, c * 128:(c + 1) * 128],
                                                                              in_=qqc[:, c * 128:(c + 1) * 128], identity=ident[:]),
                                             rd=[R_qqc, R_ident], wr=[R_PT])
                                    k.op("act", lambda e: e.activation(
                                        out=qTa[:, nh * 4:(nh + 1) * 4, j * 128:(j + 1) * 128],
                                        in_=PT[:, 0:512].rearrange("p (c t) -> p c t", c=4), func=AF.Copy),
                                        rd=[R_PT], wr=[R_qTa[j]])
                                if pend is not None:
                                    pend()
                                pend = backq
                                if g < 3:
                                    bb = kitem // 2
                                    if kitem % 2 == 0:
                                        backs[bb] = prep_front(4 * (g + 1) + bb)
                                    else:
                                        backs[bb]()
                                kitem += 1
                    pend()
                    def load_head(h):
                        i = h % 2
                        k.dma("sp", KT[i][:], kTs[h, :, :], rd=R_kTs, wr=[R_KT[i]])
                        k.dma("sp", VX[i][:, :, 0:128], vs[:, h * 128:(h + 1) * 128].rearrange("(n p) d -> p n d", p=128),
                              rd=R_vs, wr=[R_VX[i]])
                    sb_cnt = [0]

                    def att_front(h, j):
                        hi = h % 2
                        nk = 2 * j + 2
                        jp = j % 2
                        for c in range(2):
                            pts, R_pts = PTs[c][jp], R_PTs[c][jp]
                            for g0 in range(0, nk, 4):
                                ng = min(4, nk - g0)
                                bk, R_bk = PB[sb_cnt[0] % 3], R_PB[sb_cnt[0] % 3]
                                sb_cnt[0] += 1
                                for i in range(ng):
                                    kb = g0 + i
                                    k.op("pe", lambda e, i=i, kb=kb: e.matmul(bk[:, i * 128:(i + 1) * 128],
                                                                             lhsT=KT[hi][c * 64:(c + 1) * 64, kb * 128:(kb + 1) * 128],
                                                                             rhs=qTa[c * 64:(c + 1) * 64, h, j * 128:(j + 1) * 128],
                                                                             start=True, stop=True),
                                         rd=[R_KT[hi], R_qTa[j]], wr=[R_bk])
                                k.op("act", lambda e: e.activation(out=pts[:, g0 * 128:(g0 + ng) * 128], in_=bk[:, 0:ng * 128], func=AF.Exp),
                                     rd=[R_bk], wr=[R_pts])
                            k.op("pool", lambda e: e.tensor_tensor(out=pts[:, 2 * j * 128:(2 * j + 2) * 128],
                                                                   in0=pts[:, 2 * j * 128:(2 * j + 2) * 128],
                                                                   in1=amask[:], op=ALU.mult),
                                 rd=[R_pts, R_amask], wr=[R_pts])

                    def att_back(h, j):
                        hi = h % 2
                        nk = 2 * j + 2
                        jp = j % 2
                        PO = [PB[3 + 2 * jp], PB[4 + 2 * jp]]
                        R_PO = [R_PB[3 + 2 * jp], R_PB[4 + 2 * jp]]
                        for c in range(2):
                            pts, R_pts = PTs[c][jp], R_PTs[c][jp]
                            for kb in range(nk):
                                k.op("pe", lambda e, kb=kb: e.matmul(PO[c][:, 0:129], lhsT=pts[:, kb * 128:(kb + 1) * 128],
                                                                     rhs=VX[hi][:, kb, 0:129], start=(kb == 0), stop=(kb == nk - 1)),
                                     rd=[R_pts, R_VX[hi]], wr=[R_PO[c]])
                        sm_ = bsm2[jp]
                        R_s = R_bsm2[jp]
                        k.op("dve", lambda e: e.reciprocal(out=sm_[:, 0:1], in_=PO[0][:, 128:129]), rd=[R_PO[0]], wr=[R_s[0]])
                        k.op("dve", lambda e: e.reciprocal(out=sm_[:, 1:2], in_=PO[1][:, 128:129]), rd=[R_PO[1]], wr=[R_s[1]])
                        k.op("dve", lambda e: e.tensor_tensor(out=sm_[:, 2:3], in0=sm_[:, 1:2], in1=NEGLAM, op=ALU.mult),
                             rd=[R_s[1], R_NEGLAM], wr=[R_s[2]])
                        k.op("dve", lambda e: e.tensor_scalar(out=tcomb2[jp][:], in0=PO[1][:, 0:128], scalar1=sm_[:, 2:3], scalar2=None,
                                                              op0=ALU.mult), rd=[R_PO[1], R_s[2]], wr=[R_tcomb2[jp]])
                        k.op("dve", lambda e: e.scalar_tensor_tensor(out=osb2[jp][:], in0=PO[0][:, 0:128], scalar=sm_[:, 0:1],
                                                                     in1=tcomb2[jp][:], op0=ALU.mult, op1=ALU.add),
                             rd=[R_PO[0], R_s[0], R_tcomb2[jp]], wr=[R_osb2[jp]])
                        k.op("act", lambda e: e.activation(out=ojunk[:], in_=osb2[jp][:], func=AF.Square, accum_out=sm_[:, 3:4]),
                             rd=[R_osb2[jp]], wr=[R_s[3]])
                        k.op("dve", lambda e: e.tensor_scalar(out=sm_[:, 4:5], in0=sm_[:, 3:4], scalar1=1.0 / 128, scalar2=EPS,
                                                              op0=ALU.mult, op1=ALU.add), rd=[R_s[3]], wr=[R_s[4]])
                        k.op("pool", lambda e: e.tensor_tensor(out=sm_[:, 5:6], in0=sm_[:, 4:5], in1=cst[:, 12:13], op=ALU.pow),
                             rd=[R_s[4], R_cst], wr=[R_s[5]])
                        k.op("dve", lambda e: e.scalar_tensor_tensor(out=ao[:, j, h * 128:(h + 1) * 128], in0=osb2[jp][:],
                                                                     scalar=sm_[:, 5:6], in1=gsub[:], op0=ALU.mult, op1=ALU.mult),
                             rd=[R_osb2[jp], R_s[5], R_gsub], wr=[R_ao] + R_hT2)

                    load_head(0)
                    items = [(h, j) for h in range(8) for j in range(16)]
                    for idx, (h, j) in enumerate(items):
                        att_front(h, j)
                        if idx > 0:
                            att_back(*items[idx - 1])
                        if j == 0 and h + 1 < 8:
                            load_head(h + 1)
                    att_back(*items[-1])
                    k.barrier()
                with ExitStack() as pes3:
                    specs = []
                    for g in range(4):
                        for nh in range(2):
                            specs.append((("wdo", g, nh), [(0, 0, w_do[:, nh * 512:(nh + 1) * 512], 8, 512)]))
                    ws = WStream(k, specs, pes3, name="wbB3")
                    xo2 = [k.sb("xo%d" % i, [128, 4, D], F32, pes3) for i in range(2)]
                    R_xo2 = [[Reg() for _ in range(4)] for _ in range(2)]
                    aoT2 = [k.sb("aoT%d" % i, [128, 8, 512], BF16, pes3) for i in range(2)]; R_aoT2 = [Reg(), Reg()]

                    def b3_prep(g):
                        xo, R_xo, aoT, R_aoT = xo2[g % 2], R_xo2[g % 2], aoT2[g % 2], R_aoT2[g % 2]
                        k.dma("sp", xo[:], xown[g * 512:(g + 1) * 512, :].rearrange("(b p) d -> p b d", p=128),
                              rd=[R_xown[g]], wr=R_xo)
                        for b in range(4):
                            j = 4 * g + b
                            for c in range(8):
                                k.op("pe", lambda e, c=c, j=j: e.transpose(out=PT[:, c * 128:(c + 1) * 128],
                                                                           in_=ao[:, j, c * 128:(c + 1) * 128], identity=ident[:]),
                                     rd=[R_ao, R_ident], wr=[R_PT])
                            k.op("act", lambda e, b=b: e.activation(out=aoT[:, :, b * 128:(b + 1) * 128],
                                                                    in_=PT[:].rearrange("p (c t) -> p c t", c=8), func=AF.Copy),
                                 rd=[R_PT], wr=[R_aoT])

                    def b3_gemm(g):
                        xo, R_xo, aoT, R_aoT = xo2[g % 2], R_xo2[g % 2], aoT2[g % 2], R_aoT2[g % 2]
                        for nh in range(2):
                            wb, R_w = ws.next(("wdo", g, nh))
                            for b in range(4):
                                P, R_P = PB[b], R_PB[b]
                                for kc in range(8):
                                    k.op("pe", lambda e, kc=kc, P=P, wb=wb, b=b:
                                         e.matmul(P[:], lhsT=aoT[:, kc, b * 128:(b + 1) * 128], rhs=wb[:, kc, :],
                                                  start=(kc == 0), stop=(kc == 7)),
                                         rd=[R_w, R_aoT], wr=[R_P])
                                k.op("dve", lambda e, b=b, nh=nh, P=P: e.tensor_tensor(out=xo[:, b, nh * 512:(nh + 1) * 512],
                                                                                      in0=xo[:, b, nh * 512:(nh + 1) * 512], in1=P[:],
                                                                                      op=ALU.add),
                                     rd=[R_P, R_xo[b]], wr=[R_xo[b]])
                        k.dma("sp", xbs[g * 512:(g + 1) * 512, :].rearrange("(b p) d -> p b d", p=128), xo[:], rd=R_xo, wr=[R_xbs[g]])

                    b3_prep(0)
                    for g in range(4):
                        if g + 1 < 4:
                            b3_prep(g + 1)
                        b3_gemm(g)
              except _Stop:
                pass
              k.barrier()

        if "C" in phases:
            with ExitStack() as pes:
              try:
                xres = k.sb("xres", [128, 16, D], F32, pes); R_xres = [[Reg(), Reg()] for _ in range(16)]
                hT3 = k.sb("hT3", [128, 8, 2048], BF16, pes); R_hT3 = [Reg() for _ in range(4)]
                gates = k.sb("gates", [128, 16, 8], F32, pes); R_gates = Reg()
                rw = k.sb("rw", [128, 8, 8], BF16, pes); R_rw = Reg()
                actg = [k.sb("actg%d" % i, [128, 4, 2048], BF16, pes) for i in range(2)]; R_actg = [Reg(), Reg()]
                sgc = [k.sb("sgc%d" % i, [128, 512], BF16, pes) for i in range(2)]; R_sgc = [Reg(), Reg()]
                gain2 = k.sb("gain2", [128, 2, D], F32, pes); R_gain2 = Reg()
                cjunk = k.sb("cjunk", [128, D], BF16, pes); R_cjunk = Reg()
                chn = k.sb("chn", [128, D], BF16, pes); R_chn = Reg()
                csm = k.sb("csm", [128, 24], F32, pes); R_csm = [Reg() for _ in range(24)]
                chn1 = k.sb("chn1", [128, D], BF16, pes)
                csmb = k.sb("csmb", [128, 4], F32, pes)
                ctmps = [(cjunk, R_cjunk, csm[:, 0:1], R_csm[0], csm[:, 1:2], R_csm[1], csm[:, 2:3], R_csm[2], chn, R_chn),
                         (cjunk, Reg(), csmb[:, 0:1], Reg(), csmb[:, 1:2], Reg(), csmb[:, 2:3], Reg(), chn1, Reg())]
                lg = k.sb("lg", [128, 8], F32, pes); R_lg = Reg()
                ge = [k.sb("ge%d" % i, [128, 8], F32, pes) for i in range(4)]; R_ge = [Reg() for _ in range(4)]
                ost = [k.sb("ost%d" % i, [128, D], F32, pes) for i in range(2)]; R_ost = [Reg(), Reg()]
                tmpc = [k.sb("tmpc%d" % i, [128, 512], F32, pes) for i in range(2)]; R_tmpc = [Reg(), Reg()]
                bcast_load(gain2[:, 0, :], ln_ffn[1:2, :], R_gain2)
                bcast_load(gain2[:, 1, :], final_norm[0:1, :], R_gain2)
                k.dma("pool", rw[:], router.rearrange("(kc p) n -> p kc n", p=128), wr=[R_rw])
                specs = []
                for ex in range(NEXP):
                    for fg in range(7):
                        for i in range(2):
                            f0 = (fg * 4 + 2 * i) * 128
                            specs.append((("gu", ex, fg, i), [(0, 0, moe_gu[ex, :, f0:f0 + 256], 8, 256),
                                                              (0, 256, moe_gu[ex, :, EXD + f0:EXD + f0 + 256], 8, 256)]))
                        specs.append((("dn", ex, fg), [(0, 0, moe_dn[ex, fg * 512:(fg + 1) * 512, 0:512], 4, 512),
                                                       (4, 0, moe_dn[ex, fg * 512:(fg + 1) * 512, 512:1024], 4, 512)]))
                ws = WStream(k, specs, pes, nbuf=6, pf=3, name="wbC")
                for g in range(4):
                    k.dma("sp", xres[:, g * 4:(g + 1) * 4, :], xbs[g * 512:(g + 1) * 512, :].rearrange("(b p) d -> p b d", p=128),
                          rd=[R_xbs[g]], wr=[r for b in range(4) for r in R_xres[g * 4 + b]])
                def c_front(j):
                    return rmsnorm_to_hT(xres[:, j, :], R_xres[j][0], gain2[:, 0, :], R_gain2, hT3[:, :, j * 128:(j + 1) * 128],
                                         R_hT3[j // 4], ctmps[j % 2], defer=True, sq_on_dve=(j % 2 == 1))
                c_back = c_front(0)
                for j in range(16):
                    nxt_back = c_front(j + 1) if j + 1 < 16 else None
                    c_back()
                    c_back = nxt_back
                    Pl, R_Pl = PB[0], R_PB[0]
                    for kc in range(8):
                        k.op("pe", lambda e, kc=kc, j=j: e.matmul(Pl[:, 0:8], lhsT=hT3[:, kc, j * 128:(j + 1) * 128], rhs=rw[:, kc, :],
                                                                  start=(kc == 0), stop=(kc == 7)),
                             rd=[R_hT3[j // 4], R_rw], wr=[R_Pl])
                    k.op("act", lambda e: e.activation(out=lg[:], in_=Pl[:, 0:8], func=AF.Copy), rd=[R_Pl], wr=[R_lg])
                    k.op("dve", lambda e: e.reduce_max(out=csm[:, 3:4], in_=lg[:], axis=mybir.AxisListType.X), rd=[R_lg], wr=[R_csm[3]])
                    k.op("dve", lambda e: e.tensor_scalar(out=ge[0][:], in0=lg[:], scalar1=csm[:, 3:4], scalar2=None, op0=ALU.is_equal),
                         rd=[R_lg, R_csm[3]], wr=[R_ge[0]])
                    k.op("dve", lambda e: e.scalar_tensor_tensor(out=ge[1][:], in0=ge[0][:], scalar=-1e30, in1=lg[:],
                                                                 op0=ALU.mult, op1=ALU.add), rd=[R_ge[0], R_lg], wr=[R_ge[1]])
                    k.op("dve", lambda e: e.reduce_max(out=csm[:, 4:5], in_=ge[1][:], axis=mybir.AxisListType.X), rd=[R_ge[1]], wr=[R_csm[4]])
                    k.op("dve", lambda e: e.tensor_scalar(out=ge[2][:], in0=ge[1][:], scalar1=csm[:, 4:5], scalar2=None, op0=ALU.is_equal),
                         rd=[R_ge[1], R_csm[4]], wr=[R_ge[2]])
                    k.op("dve", lambda e: e.tensor_tensor(out=csm[:, 5:6], in0=csm[:, 4:5], in1=csm[:, 3:4], op=ALU.subtract),
                         rd=[R_csm[3], R_csm[4]], wr=[R_csm[5]])
                    k.op("act", lambda e: e.activation(out=csm[:, 6:7], in_=csm[:, 5:6], func=AF.Sigmoid), rd=[R_csm[5]], wr=[R_csm[6]])
                    k.op("dve", lambda e: e.tensor_scalar(out=csm[:, 7:8], in0=csm[:, 6:7], scalar1=-1.0, scalar2=1.0,
                                                          op0=ALU.mult, op1=ALU.add), rd=[R_csm[6]], wr=[R_csm[7]])
                    k.op("dve", lambda e: e.tensor_scalar(out=ge[3][:], in0=ge[0][:], scalar1=csm[:, 7:8], scalar2=None, op0=ALU.mult),
                         rd=[R_ge[0], R_csm[7]], wr=[R_ge[3]])
                    k.op("dve", lambda e, j=j: e.scalar_tensor_tensor(out=gates[:, j, :], in0=ge[2][:], scalar=csm[:, 6:7], in1=ge[3][:],
                                                                     op0=ALU.mult, op1=ALU.add),
                         rd=[R_ge[2], R_ge[3], R_csm[6]], wr=[R_gates])
                stage(30)
                gcnt = 0
                dcnt = [0]
                pending = []

                def make_down_units(ex, ab, R_ab, wd, R_wd):
                    units = []
                    for b in range(16):
                        for nh in range(2):
                            def unit(b=b, nh=nh):
                                P, R_P = PB[dcnt[0] % 3], R_PB[dcnt[0] % 3]
                                dcnt[0] += 1
                                for fcl in range(4):
                                    k.op("pe", lambda e, fcl=fcl: e.matmul(P[:], lhsT=ab[:, fcl, b * 128:(b + 1) * 128],
                                                                           rhs=wd[:, nh * 4 + fcl, :], start=(fcl == 0), stop=(fcl == 3)),
                                         rd=[R_wd, R_ab], wr=[R_P])
                                k.op("dve", lambda e: e.scalar_tensor_tensor(
                                    out=xres[:, b, nh * 512:(nh + 1) * 512], in0=P[:], scalar=gates[:, b, ex:ex + 1],
                                    in1=xres[:, b, nh * 512:(nh + 1) * 512], op0=ALU.mult, op1=ALU.add),
                                    rd=[R_P, R_gates, R_xres[b][nh]], wr=[R_xres[b][nh]])
                            units.append(unit)
                    return units

                for ex in range(NEXP):
                    for fg in range(7):
                        ab, R_ab = actg[(ex * 7 + fg) % 2], R_actg[(ex * 7 + fg) % 2]
                        for i in range(2):
                            wb, R_w = ws.next(("gu", ex, fg, i))
                            for fi in range(2):
                                fcl = 2 * i + fi
                                for q in range(4):
                                    pp = gcnt % 2
                                    gcnt += 1
                                    Pg, R_Pg = PB[3 + 2 * pp], R_PB[3 + 2 * pp]
                                    Pu, R_Pu = PB[4 + 2 * pp], R_PB[4 + 2 * pp]
                                    for P, R_P, c0 in ((Pg, R_Pg, fi * 128), (Pu, R_Pu, 256 + fi * 128)):
                                        for kc in range(8):
                                            k.op("pe", lambda e, kc=kc, P=P, c0=c0, wb=wb, q=q:
                                                 e.matmul(P[:], lhsT=wb[:, kc, c0:c0 + 128], rhs=hT3[:, kc, q * 512:(q + 1) * 512],
                                                          start=(kc == 0), stop=(kc == 7)),
                                                 rd=[R_w, R_hT3[q]], wr=[R_P])
                                    k.op("act", lambda e, pp=pp, Pg=Pg: e.activation(out=sgc[pp][:], in_=Pg[:], func=AF.Silu),
                                         rd=[R_Pg], wr=[R_sgc[pp]])
                                    k.op("dve", lambda e, pp=pp, Pu=Pu, fcl=fcl, q=q, ab=ab:
                                         e.tensor_tensor(out=ab[:, fcl, q * 512:(q + 1) * 512], in0=Pu[:], in1=sgc[pp][:], op=ALU.mult),
                                         rd=[R_Pu, R_sgc[pp]], wr=[R_ab])
                                    for _ in range(2):
                                        if pending:
                                            pending.pop(0)()
                        wd, R_wd = ws.next(("dn", ex, fg))
                        assert not pending
                        pending = make_down_units(ex, ab, R_ab, wd, R_wd)
                    stage(31 + ex)
                def final_norm(j):
                    o_t, R_o = ost[j % 2], R_ost[j % 2]
                    k.op("act", lambda e: e.activation(out=cjunk[:], in_=xres[:, j, :], func=AF.Square, accum_out=csm[:, 8:9]),
                         rd=R_xres[j], wr=[R_cjunk, R_csm[8]])
                    k.op("dve", lambda e: e.tensor_scalar(out=csm[:, 9:10], in0=csm[:, 8:9], scalar1=1.0 / D, scalar2=EPS,
                                                          op0=ALU.mult, op1=ALU.add), rd=[R_csm[8]], wr=[R_csm[9]])
                    k.op("pool", lambda e: e.tensor_tensor(out=csm[:, 10:11], in0=csm[:, 9:10], in1=cst[:, 12:13], op=ALU.pow),
                         rd=[R_csm[9], R_cst], wr=[R_csm[10]])
                    k.op("dve", lambda e: e.scalar_tensor_tensor(out=o_t[:], in0=xres[:, j, :], scalar=csm[:, 10:11],
                                                                 in1=gain2[:, 1, :], op0=ALU.mult, op1=ALU.mult),
                         rd=R_xres[j] + [R_csm[10], R_gain2], wr=[R_o])
                    k.dma("sp", out[j * 128:(j + 1) * 128, :], o_t[:], rd=[R_o], wr=[R_out])

                assert len(pending) == 32
                for j in range(16):
                    pending.pop(0)()
                    pending.pop(0)()
                    if j >= 1:
                        final_norm(j - 1)
                final_norm(15)
              except _Stop:
                pass
              k.barrier()

        outs = [R_out] if R_out.dsem is not None else []
        if debug:
            outs += [r for r in (R_x1s + R_kTs + R_vs + R_xbs) if r.dsem is not None]
        k.finish(outs)
        print("ops", k.nops, "waits", k.nwaits, "sig", k.nsig, "dsems", k.nsem)
    _DECL[id(nc)] = list(declared)
    return nc


_DECL = {}


def make_in_maps(inputs):
    HC = host_consts()
    x = np.asarray(inputs["x"], np.float32)
    f = lambda a: np.ascontiguousarray(np.asarray(a, np.float32))
    shared = {
        "ln_mix": f(inputs["ln_mix"]), "ln_ffn": f(inputs["ln_ffn"]),
        "ret_w_in": f(inputs["ret_w_in"][0]), "ret_w_o": f(inputs["ret_w_o"][0]),
        "kv_norm": f(inputs["kv_norm"]).reshape(1, D), "w_kv": f(inputs["w_kv"]),
        "diff_w_q": f(inputs["diff_w_q"][0]),
        "lam": np.concatenate([f(inputs["lam_q1"][0]), f(inputs["lam_k1"][0]),
                               f(inputs["lam_q2"][0]), f(inputs["lam_k2"][0])]).reshape(1, 256),
        "diff_subln": f(inputs["diff_subln"]).reshape(1, 128), "diff_w_o": f(inputs["diff_w_o"][0]),
        "ffn_w_gu": f(inputs["ffn_w_gu"][0]), "ffn_w_down": f(inputs["ffn_w_down"][0]),
        "moe_router": f(inputs["moe_router"][0]), "moe_w_gu": f(inputs["moe_w_gu"][0]),
        "moe_w_down": f(inputs["moe_w_down"][0]), "final_norm": f(inputs["final_norm"]).reshape(1, D),
        "rcos": HC["rcos"], "rsin": HC["rsin"], "dcs": HC["dcs"], "maskT": HC["maskT"], "ident": HC["ident"],
    }
    maps = []
    kq = np.arange(128)
    diag = ((kq[:, None] // 64) <= (kq[None, :] // 64)).astype(np.float32)
    for c in range(NCORE):
        b, h = c // 2, c % 2
        m = dict(shared)
        m["x"] = np.ascontiguousarray(x[b])
        cst = HC["cst"].copy()
        cst[:, 14] = 1.0 if h == 0 else 0.0
        cst[:, 15] = 1.0 if h == 1 else 0.0
        m["cst"] = cst
        own = np.concatenate([np.arange((2 * j + h) * 128, (2 * j + h + 1) * 128) for j in range(16)])
        m["dcs_own"] = np.ascontiguousarray(HC["dcs"][own])
        am = np.zeros((128, 2, 128), np.float32)
        if h == 0:
            am[:, 0, :] = diag
        else:
            am[:, 0, :] = 1.0
            am[:, 1, :] = diag
        m["amask"] = am
        maps.append(m)
    return maps


_NC_CACHE = {}


def kernel(**inputs):
    if "nc" not in _NC_CACHE:
        _NC_CACHE["nc"] = build_program()
    nc = _NC_CACHE["nc"]
    maps = make_in_maps(inputs)
    res = run_bass_kernel_spmd(nc, maps, core_ids=list(range(NCORE)))
    out = np.zeros((4, S, D), np.float32)
    for c in range(NCORE):
        b, h = c // 2, c % 2
        o = np.asarray(res.results[c]["out"]).reshape(16, 128, D)
        for j in range(16):
            out[b, (2 * j + h) * 128:(2 * j + h + 1) * 128] = o[j]
    return out
```
